# Optimizing a Trainium2 kernel written in Bass

```python
import math
import jax, jax.numpy as jnp
from jax import lax
import numpy as np

D_MODEL = 2048
BATCH = 2
SEQ = 16384
DEPTH = 4

HEAD_DIM = 64
MIX_WIDTH = D_MODEL // 4
BRANCH_W = MIX_WIDTH // 2
N_HEADS = BRANCH_W // HEAD_DIM
N_IN_PARTS = 8
ROPE_THETA = 10000.0
DIL_PAIRS = ((128, 1), (512, 4), (2048, 16))
DIL_STEPS = 128
MOBA_BLOCK = 256
MOBA_TOPK = 3
DIFF_HEAD_DIM = HEAD_DIM // 2
Q_BLOCK = 128
RMS_EPS = 1e-6
N_ODD = DEPTH // 2

kernel_name = "hybrid_dilated_moba_diff_stickbreak"


def _rmsnorm(x, g):
    xf = x.astype(jnp.float32)
    y = xf * lax.rsqrt(jnp.mean(xf * xf, axis=-1, keepdims=True) + RMS_EPS)
    return (y * g.astype(jnp.float32)).astype(x.dtype)


def _rope(t, pos):
    d = t.shape[-1]
    half = d // 2
    inv = ROPE_THETA ** (-jnp.arange(half, dtype=jnp.float32) / half)
    ang = pos.astype(jnp.float32)[:, None] * inv[None, :]
    cos, sin = jnp.cos(ang), jnp.sin(ang)
    tf = t.astype(jnp.float32)
    t1, t2 = tf[..., :half], tf[..., half:]
    return jnp.concatenate([t1 * cos - t2 * sin, t1 * sin + t2 * cos], axis=-1).astype(t.dtype)


def _heads(t):
    b, s, _ = t.shape
    return t.reshape(b, s, N_HEADS, -1).transpose(0, 2, 1, 3)


def _merge(t):
    b, h, s, d = t.shape
    return t.transpose(0, 2, 1, 3).reshape(b, s, h * d)


def _dilated_branch(q, k, v, r):
    b, h, s, d = q.shape
    L = -(-s // (r * DIL_STEPS)) * DIL_STEPS
    pad = L * r - s
    nb = L // DIL_STEPS

    def to_phase(t):
        t = jnp.pad(t, ((0, 0), (0, 0), (0, pad), (0, 0)))
        t = t.reshape(b, h, L, r, d).transpose(0, 1, 3, 2, 4)
        return t.reshape(b, h, r, nb, DIL_STEPS, d)

    qb, kb, vb = to_phase(q), to_phase(k), to_phase(v)
    prev = lambda t: jnp.pad(t, ((0, 0), (0, 0), (0, 0), (1, 0), (0, 0), (0, 0)))[:, :, :, :-1]
    kk = jnp.concatenate([prev(kb), kb], axis=4)
    vv = jnp.concatenate([prev(vb), vb], axis=4)
    sc = jnp.einsum('bhrnqd,bhrnkd->bhrnqk', qb, kk).astype(jnp.float32) / math.sqrt(d)
    i = jnp.arange(DIL_STEPS)[:, None]
    j = jnp.arange(2 * DIL_STEPS)[None, :]
    band = (j >= i) & (j <= i + DIL_STEPS)
    exists = (jnp.arange(nb)[:, None, None] > 0) | (j[None] >= DIL_STEPS)
    mask = band[None] & exists
    sc = jnp.where(mask, sc, -jnp.inf)
    m = jnp.max(sc, axis=-1, keepdims=True)
    p = jnp.exp(sc - m)
    den = jnp.sum(p, axis=-1)
    o = jnp.einsum('bhrnqk,bhrnkd->bhrnqd', p, vv.astype(jnp.float32)) / den[..., None]
    lse = m[..., 0] + jnp.log(den)
    o = o.reshape(b, h, r, L, d).transpose(0, 1, 3, 2, 4).reshape(b, h, L * r, d)[:, :, :s]
    lse = lse.reshape(b, h, r, L).transpose(0, 1, 3, 2).reshape(b, h, L * r)[:, :, :s]
    return o, lse


def _dilated_mixture(q, k, v):
    outs, lses = [], []
    for _, r in DIL_PAIRS:
        o, l = _dilated_branch(q, k, v, r)
        outs.append(o)
        lses.append(l)
    w = jax.nn.softmax(jnp.stack(lses, axis=0), axis=0)[..., None]
    return jnp.sum(w * jnp.stack(outs, axis=0), axis=0).astype(q.dtype)


def _moba(q, k, v):
    b, h, s, d = q.shape
    sp = -(-s // MOBA_BLOCK) * MOBA_BLOCK
    nblk = sp // MOBA_BLOCK
    kp = jnp.pad(k, ((0, 0), (0, 0), (0, sp - s), (0, 0)))
    vp = jnp.pad(v, ((0, 0), (0, 0), (0, sp - s), (0, 0)))
    kb = kp.reshape(b, h, nblk, MOBA_BLOCK, d)
    vb = vp.reshape(b, h, nblk, MOBA_BLOCK, d)
    kmean = jnp.mean(kb.astype(jnp.float32), axis=3)
    n_sel = min(MOBA_TOPK, nblk)
    nch = s // Q_BLOCK
    qc = q.reshape(b, h, nch, Q_BLOCK, d).transpose(2, 0, 1, 3, 4)
    starts = jnp.arange(nch, dtype=jnp.int32) * Q_BLOCK
    b_ix = jnp.arange(b)[:, None, None, None]
    h_ix = jnp.arange(h)[None, :, None, None]
    scale = 1.0 / math.sqrt(d)
    kpos_blk = jnp.arange(MOBA_BLOCK)

    def chunk(args):
        qch, t0 = args
        cur = t0 // MOBA_BLOCK
        gate = jnp.einsum('bhqd,bhnd->bhqn', qch.astype(jnp.float32), kmean)
        gate = jnp.where(jnp.arange(nblk) < cur, gate, -jnp.inf)
        _, sel = lax.top_k(gate, n_sel)
        kg = kb[b_ix, h_ix, sel]
        vg = vb[b_ix, h_ix, sel]
        k_own = lax.dynamic_slice_in_dim(kp, cur * MOBA_BLOCK, MOBA_BLOCK, axis=2)
        v_own = lax.dynamic_slice_in_dim(vp, cur * MOBA_BLOCK, MOBA_BLOCK, axis=2)
        s_sel = jnp.einsum('bhqd,bhqnkd->bhqnk', qch, kg).astype(jnp.float32) * scale
        s_sel = jnp.where((jnp.arange(n_sel) < cur)[:, None], s_sel, -jnp.inf)
        s_own = jnp.einsum('bhqd,bhkd->bhqk', qch, k_own).astype(jnp.float32) * scale
        qpos = t0 + jnp.arange(Q_BLOCK)
        s_own = jnp.where(cur * MOBA_BLOCK + kpos_blk[None, :] <= qpos[:, None], s_own, -jnp.inf)
        n_sk = n_sel * MOBA_BLOCK
        p = jax.nn.softmax(jnp.concatenate([s_sel.reshape(b, h, Q_BLOCK, n_sk), s_own], axis=-1), axis=-1)
        p_sel = p[..., :n_sk].reshape(b, h, Q_BLOCK, n_sel, MOBA_BLOCK).astype(v.dtype)
        p_own = p[..., n_sk:].astype(v.dtype)
        return (jnp.einsum('bhqnk,bhqnkd->bhqd', p_sel, vg)
                + jnp.einsum('bhqk,bhkd->bhqd', p_own, v_own))

    out = lax.map(chunk, (qc, starts))
    return out.transpose(1, 2, 0, 3, 4).reshape(b, h, s, d)


def _diff_attention(q, k, v, lam, subln_g, lambda_init):
    s = k.shape[-2]
    scale = 1.0 / math.sqrt(q.shape[-1])
    lf = lam.astype(jnp.float32)
    lam_val = jnp.exp(jnp.sum(lf[0] * lf[1])) - jnp.exp(jnp.sum(lf[2] * lf[3])) + lambda_init
    outs = []
    for t0 in range(0, s, Q_BLOCK):
        kl = t0 + Q_BLOCK
        sc = jnp.einsum('bhiqd,bhikd->bhiqk', q[:, :, :, t0:kl], k[:, :, :, :kl]).astype(jnp.float32) * scale
        causal = jnp.arange(kl)[None, :] <= (t0 + jnp.arange(Q_BLOCK))[:, None]
        p = jax.nn.softmax(jnp.where(causal, sc, -jnp.inf), axis=-1)
        a = p[:, :, 0] - lam_val * p[:, :, 1]
        outs.append(jnp.einsum('bhqk,bhkd->bhqd', a.astype(v.dtype), v[:, :, :kl]))
    o = _rmsnorm(jnp.concatenate(outs, axis=2), subln_g)
    return (o.astype(jnp.float32) * (1.0 - lambda_init)).astype(v.dtype)


def _stick_breaking(q, k, v):
    b, h, s, d = q.shape
    scale = 1.0 / math.sqrt(d)
    ar = jnp.arange(Q_BLOCK)
    tri = (ar[:, None] > ar[None, :]).astype(jnp.float32)
    outs = []
    for i, t0 in enumerate(range(0, s, Q_BLOCK)):
        nc = i + 1
        kl = nc * Q_BLOCK
        z = jnp.einsum('bhqd,bhkd->bhqk', q[:, :, t0:kl], k[:, :, :kl]).astype(jnp.float32) * scale
        before = jnp.arange(kl)[None, :] < (t0 + ar)[:, None]
        ln1m = jnp.where(before, jax.nn.log_sigmoid(-z), 0.0)
        ln1m_c = ln1m.reshape(b, h, Q_BLOCK, nc, Q_BLOCK)
        tri_c = (jnp.arange(nc)[:, None] > jnp.arange(nc)[None, :]).astype(jnp.float32)
        tail = (jnp.einsum('bhqcj,js->bhqcs', ln1m_c, tri)
                + jnp.einsum('bhqe,ec->bhqc', jnp.sum(ln1m_c, axis=-1), tri_c)[..., None])
        log_a = jnp.where(before, z + ln1m + tail.reshape(b, h, Q_BLOCK, kl), -jnp.inf)
        outs.append(jnp.einsum('bhqk,bhkd->bhqd', jnp.exp(log_a).astype(v.dtype), v[:, :, :kl]))
    return jnp.concatenate(outs, axis=2)


def setup_inputs(seed: int = 0) -> dict:
    key = jax.random.key(seed)
    ks = jax.random.split(key, 8)
    x = jax.random.normal(ks[0], (BATCH, SEQ, D_MODEL), jnp.float32)
    norm_g = 1.0 + 0.02 * jax.random.normal(ks[1], (DEPTH, D_MODEL), jnp.float32)
    w_in = jax.random.normal(ks[2], (DEPTH, D_MODEL, N_IN_PARTS * BRANCH_W), jnp.float32) * D_MODEL ** -0.5
    w_out = jax.random.normal(ks[3], (DEPTH, MIX_WIDTH, D_MODEL), jnp.float32) * MIX_WIDTH ** -0.5
    diff_lam = 0.1 * jax.random.normal(ks[4], (N_ODD, 4, DIFF_HEAD_DIM), jnp.float32)
    diff_subln_g = 1.0 + 0.02 * jax.random.normal(ks[5], (N_ODD, HEAD_DIM), jnp.float32)
    final_norm_g = 1.0 + 0.02 * jax.random.normal(ks[6], (D_MODEL,), jnp.float32)
    return {"x": x, "norm_g": norm_g, "w_in": w_in, "w_out": w_out,
            "diff_lam": diff_lam, "diff_subln_g": diff_subln_g,
            "final_norm_g": final_norm_g}


def reference(x, norm_g, w_in, w_out, diff_lam, diff_subln_g, final_norm_g):
    b, s, _ = x.shape
    pos = jnp.arange(s)
    for layer in range(DEPTH):
        h = _rmsnorm(x, norm_g[layer])
        proj = jnp.einsum('bsd,de->bse', h, w_in[layer])
        parts = jnp.split(proj, N_IN_PARTS, axis=-1)
        if layer % 2 == 0:
            qa, ka, va, ga, qb, kb, vb, gb = parts
            oa = _dilated_mixture(_rope(_heads(qa), pos), _rope(_heads(ka), pos), _heads(va))
            ob = _moba(_rope(_heads(qb), pos), _rope(_heads(kb), pos), _heads(vb))
            y = jnp.concatenate([_merge(oa) * jax.nn.silu(ga),
                                 _merge(ob) * jax.nn.silu(gb)], axis=-1)
        else:
            qc, kc, vc, gc, qd, kd, vd, gd = parts
            li = layer // 2
            lambda_init = 0.8 - 0.6 * math.exp(-0.3 * layer)
            split2 = lambda t: t.reshape(b, s, N_HEADS, 2, DIFF_HEAD_DIM).transpose(0, 2, 3, 1, 4)
            oc = _diff_attention(_rope(split2(qc), pos), _rope(split2(kc), pos), _heads(vc),
                                 diff_lam[li], diff_subln_g[li], lambda_init)
            od = _stick_breaking(_heads(qd), _heads(kd), _heads(vd))
            y = jnp.concatenate([_merge(oc) * jax.nn.silu(gc),
                                 _merge(od) * jax.nn.silu(gd)], axis=-1)
        x = x + jnp.einsum('bse,ed->bsd', y.astype(x.dtype), w_out[layer])
    return _rmsnorm(x, final_norm_g)
```

```python
import contextlib
import math

import ml_dtypes
import numpy as np

import concourse.bass as bass
import concourse.mybir as mybir
from concourse.bass_utils import run_bass_kernel_spmd

F32 = mybir.dt.float32
BF16 = mybir.dt.bfloat16
AF = mybir.ActivationFunctionType
ALU = mybir.AluOpType
AX = mybir.AxisListType
NPBF = ml_dtypes.bfloat16

D_MODEL = 2048
SEQ = 16384
DEPTH = 4
CH = 512
KC = 16
EPS = 1e-6
VW = 72
NEG = -30000.0


class Sched:
    SEG = 16000

    def __init__(self, nc, stack):
        self.nc = nc
        self.stack = stack
        self.eng = {"pe": nc.tensor, "act": nc.scalar, "dve": nc.vector, "pool": nc.gpsimd, "sp": nc.sync}
        self.cnt = {e: 0 for e in self.eng}
        self.sems = {e: [] for e in self.eng}
        self.waited = {e: {} for e in self.eng}
        self.lastw = {}
        self.reads = {}
        self.dsem = {}
        self.dcnt = {}
        self.n_inst = 0

    def _sem(self, e, seg):
        while len(self.sems[e]) <= seg:
            self.sems[e].append(self.stack.enter_context(self.nc.semaphore(f"s_{e}_{len(self.sems[e])}")))
        return self.sems[e][seg]

    def _wait(self, e, prod, count):
        if count <= 0:
            return
        w = self.waited[e]
        if w.get(prod, 0) >= count:
            return
        w[prod] = count
        eng = self.eng[e]
        if prod.startswith("dma:"):
            eng.wait_ge(self.dsem[prod], count)
        else:
            seg = (count - 1) // self.SEG
            eng.wait_ge(self._sem(prod, seg), (count - 1) % self.SEG + 1)

    def _deps(self, e, reads, writes):
        for r in reads:
            lw = self.lastw.get(r)
            if lw is not None and not (lw[0] == e and e == "pe"):
                self._wait(e, lw[0], lw[1])
        for wbuf in writes:
            lw = self.lastw.get(wbuf)
            if lw is not None and lw[0] != e:
                self._wait(e, lw[0], lw[1])
            for prod, cnt in self.reads.get(wbuf, {}).items():
                if prod != e:
                    self._wait(e, prod, cnt)

    def op(self, e, fn, reads=(), writes=()):
        self._deps(e, reads, writes)
        inst = fn(self.eng[e])
        self.cnt[e] += 1
        n = self.cnt[e]
        seg = (n - 1) // self.SEG
        inst.then_inc(self._sem(e, seg), 1)
        self.n_inst += 1
        for r in reads:
            self.reads.setdefault(r, {})[e] = n
        for wbuf in writes:
            self.lastw[wbuf] = (e, n)
            self.reads[wbuf] = {}

    def dma(self, q, semname, out, in_, reads=(), writes=()):
        prod = "dma:" + semname
        if prod not in self.dsem:
            self.dsem[prod] = self.stack.enter_context(self.nc.semaphore("d_" + semname))
            self.dcnt[prod] = 0
        self._deps(q, reads, writes)
        self.eng[q].dma_start(out=out, in_=in_).then_inc(self.dsem[prod], 16)
        self.dcnt[prod] += 16
        n = self.dcnt[prod]
        self.n_inst += 1
        for r in reads:
            self.reads.setdefault(r, {})[prod] = n
        for wbuf in writes:
            self.lastw[wbuf] = (prod, n)
            self.reads[wbuf] = {}

    def finish(self, q="sp"):
        for prod, n in self.dcnt.items():
            self._wait(q, prod, n)
        for e in self.eng:
            if e != q:
                self._wait(q, e, self.cnt[e])


def _rope_tables(even, seq):
    pos = np.arange(seq, dtype=np.float32)
    cos = np.ones((128, seq), np.float32)
    sin = np.zeros((128, seq), np.float32)
    rm = np.zeros((128, 128), np.float32)
    for p in range(128):
        if even:
            base, i, hd = (p // 64) * 64, p % 64, 64
        elif p < 64:
            base, i, hd = (p // 32) * 32, p % 32, 32
        else:
            rm[p, p] = 1.0
            continue
        half = hd // 2
        inv = (np.float32(10000.0) ** (-(np.arange(half, dtype=np.float32) / np.float32(half)))).astype(np.float32)
        ang = (pos * inv[i % half]).astype(np.float32)
        cos[p] = np.cos(ang).astype(np.float32)
        sgn = -1.0 if i < half else 1.0
        sin[p] = (sgn * np.sin(ang)).astype(np.float32)
        partner = base + (i + half) % hd
        rm[partner, p] = 1.0
    return cos, sin, rm


def _consts(even):
    ki = np.arange(128)[:, None]
    qi = np.arange(512)[None, :]
    c = {}
    incl = np.stack([((128 * i + ki) <= qi) for i in range(4)], 1).astype(np.float32)
    strict = np.stack([((128 * i + ki) < qi) for i in range(4)], 1).astype(np.float32)
    c["mincl"] = incl.astype(NPBF)
    c["mstrict"] = strict.astype(NPBF)
    if even:
        dm = []
        for o in range(20):
            d = 2048 - 128 * o + qi - ki
            m = ((d >= 0) & (d <= 128)).astype(np.float32)
            m += ((d >= 0) & (d % 4 == 0) & (d <= 512)).astype(np.float32)
            m += ((d >= 0) & (d % 16 == 0) & (d <= 2048)).astype(np.float32)
            dm.append(m)
        assert all(np.array_equal(dm[4], dm[o]) for o in range(4, 12))
        dm = dm[0:5] + dm[12:20]
        c["mdil"] = np.stack(dm, 1).astype(NPBF)
        e = np.zeros((128, 64, 128), np.float32)
        for blk in range(64):
            e[64 + blk, blk, :] = 1.0
        c["esel"] = e.astype(NPBF)
    else:
        j = np.arange(128)[:, None]
        s = np.arange(128)[None, :]
        c["ntri"] = (-(j >= s).astype(np.float32)).astype(NPBF)
        c["nones"] = (-np.ones((128, 128), np.float32)).astype(NPBF)
    c["onesb"] = np.ones((128, 128), np.float32).astype(NPBF)
    c["identb"] = np.eye(128, dtype=np.float32).astype(NPBF)
    sel = np.zeros((128, 64), np.float32)
    sel[64, :] = 1.0
    c["sel65"] = sel
    c["ones64"] = np.ones((64, 64), np.float32)
    return c


def build_layer(layer, n_prev, seq=SEQ):
    even = layer % 2 == 0
    nch = seq // CH
    ntile = seq // 128
    nc = bass.Bass("TRN2", target_bir_lowering=False)

    def din(name, shape, dt):
        return nc.dram_tensor(name, shape, dt, kind="ExternalInput").ap()

    xT = din("xT", [D_MODEL, seq], F32)
    w_d = din("w", [D_MODEL, 512], F32)
    ng_d = din("ng", [128, KC], F32)
    cos_d = din("cos", [128, seq], F32)
    sin_d = din("sin", [128, seq], F32)
    rm_d = din("rm", [128, 128], F32)
    mincl_d = din("mincl", [128, 4, 512], BF16)
    mstrict_d = din("mstrict", [128, 4, 512], BF16)
    onesb_d = din("onesb", [128, 128], BF16)
    identb_d = din("identb", [128, 128], BF16)
    sel65_d = din("sel65", [128, 64], F32)
    ones64_d = din("ones64", [64, 64], F32)
    if even:
        mdil_d = din("mdil", [128, 13, 512], BF16)
        esel_d = din("esel", [128, 64, 128], BF16)
    else:
        ntri_d = din("ntri", [128, 128], BF16)
        nones_d = din("nones", [128, 128], BF16)
        lam_d = din("lam", [1, 128], F32)
        subg_d = din("subg", [64, 1], F32)
    yp_d = [din(f"yp{l}", [512, seq], BF16) for l in range(n_prev)]
    wo_d = [din(f"wo{l}", [512, D_MODEL], F32) for l in range(n_prev)]
    y_out = nc.dram_tensor("y", [128, seq], BF16, kind="ExternalOutput").ap()

    with contextlib.ExitStack() as st:
        def sb(name, shape, dt):
            return st.enter_context(nc.sbuf_tensor(name, shape, dt))

        def ps(name, shape, dt):
            return st.enter_context(nc.psum_tensor(name, shape, dt))

        S = Sched(nc, st)

        Wg = sb("Wg", [128, KC, 512], BF16)
        Wo = [sb(f"Wo{l}", [128, 4, D_MODEL], BF16) for l in range(n_prev)]
        KT = sb("KT", [128, seq], BF16)
        V1 = sb("V1", [128, ntile, VW], BF16)
        V2 = sb("V2", [128, ntile, VW], BF16)
        xc = sb("xc", [128, KC, CH], F32)
        wstage = sb("wstage", [128, 2048], F32) if n_prev else None
        ngs = sb("ngs", [128, KC], F32)
        rm = sb("rm_s", [128, 128], F32)
        mincl = sb("mincl_s", [128, 4, 512], BF16)
        mstrict = sb("mstrict_s", [128, 4, 512], BF16) if not even else None
        onesb = sb("onesb_s", [128, 128], BF16)
        identb = sb("identb_s", [128, 128], BF16)
        sel65 = sb("sel65_s", [128, 64], F32)
        ones64 = sb("ones64_s", [64, 64], F32)
        if even:
            mdil = sb("mdil_s", [128, 13, 512], BF16)
            esel = sb("esel_s", [128, 64, 128], BF16)
            kmT = sb("kmT", [128, 64], F32)
            gsb = sb("gsb", [128, 64], F32)
            biasq = sb("biasq", [128, 64], BF16)
            biasT = sb("biasT", [128, 512], BF16)
            max8 = sb("max8", [128, 8], F32)
        else:
            ntri = sb("ntri_s", [128, 128], BF16)
            nones = sb("nones_s", [128, 128], BF16)
            lam = sb("lam_s", [1, 128], F32)
            lamw = sb("lamw", [1, 8], F32)
            neglam = sb("neglam", [64, 1], F32)
            subg = sb("subg_s", [64, 1], F32)
            subgs = sb("subgs", [64, 1], F32)
            ones1 = sb("ones1", [1, 64], F32)
            e1 = [sb(f"e1_{i}", [128, 512], F32) for i in range(2)]
            spb = [sb(f"spb_{i}", [128, 512], BF16) for i in range(3)]
            spsum = [sb(f"spsum_{i}", [128, 512], BF16) for i in range(2)]
        ypc = [sb(f"ypc{l}", [128, 4, CH], BF16) for l in range(n_prev)]
        sq = [sb(f"sq{i}", [128, CH], BF16) for i in range(3)]
        xb = [sb(f"xb{i}", [128, CH], BF16) for i in range(3)]
        cosc = sb("cosc", [128, CH], F32)
        sinc = sb("sinc", [128, CH], F32)
        rstd = sb("rstd", [128, CH], F32)
        tq = sb("tq", [128, CH], F32)
        ta = sb("ta", [128, CH], F32)
        tb = sb("tb", [128, CH], F32)
        qr = sb("qr", [128, CH], F32)
        kr = sb("kr", [128, CH], F32)
        vbs = sb("vbs", [128, CH], BF16)
        gate = sb("gate", [128, CH], BF16)
        gate2 = sb("gate2", [64, CH], BF16)
        nq = 2 if even else 3
        Qp = [sb(f"Qp{i}", [128, CH], BF16) for i in range(nq)]
        Pb = [sb(f"Pb{i}", [128, CH], BF16) for i in range(4)]
        osb = [sb(f"osb{i}", [128, CH], F32) for i in range(2)]
        rden = [sb(f"rden{i}", [64, CH], F32) for i in range(2)]
        on = [sb(f"on{i}", [64, CH], F32) for i in range(2)]
        ych = [sb(f"ych{i}", [64, CH], BF16) for i in range(2)]

        B = [ps(f"B{i}", [128, 512], F32) for i in range(7)]
        BT = ps("BT", [128, 4, 128], BF16)

        def load(semname, dst, dst_key, src):
            S.dma("sp", semname, dst, src, writes=[dst_key])

        load("c0", rm[:], "rm", rm_d[:, :])
        load("c1", mincl[:], "mincl", mincl_d[:, :, :])
        if not even:
            load("c2", mstrict[:], "mstrict", mstrict_d[:, :, :])
        load("c3", onesb[:], "onesb", onesb_d[:, :])
        load("c4", identb[:], "identb", identb_d[:, :])
        load("c5", sel65[:], "sel65", sel65_d[:, :])
        load("c6", ones64[:], "ones64", ones64_d[:, :])
        load("c7", ngs[:], "ngs", ng_d[:, :])
        if even:
            for o, o2 in ((0, 5), (5, 9), (9, 13)):
                load(f"c8_{o}", mdil[:, o:o2, :], ("mdil", o), mdil_d[:, o:o2, :])
            for o in range(0, 64, 16):
                load(f"c9_{o}", esel[:, o:o + 16, :], ("esel", o), esel_d[:, o:o + 16, :])
        else:
            load("c8", ntri[:], "ntri", ntri_d[:, :])
            load("c9", nones[:], "nones", nones_d[:, :])
            load("c10", lam[:], "lam", lam_d[:, :])
            load("c11", subg[:], "subg", subg_d[:, :])
        mdil_keys = [("mdil", o) for o in (0, 5, 9)]
        esel_keys = [("esel", o) for o in range(0, 64, 16)]

        w_v = w_d.rearrange("(k p) n -> p k n", p=128)
        for k0 in range(0, KC, 4):
            S.dma("sp", f"wst{k0}", xc[:, k0:k0 + 4, :], w_v[:, k0:k0 + 4, :], writes=[("xc", k0 // 4)])
            for k in range(k0, k0 + 4):
                S.op("dve", lambda e, k=k: e.tensor_scalar(
                    out=Wg[:, k, :], in0=xc[:, k, :],
                    scalar1=ngs[:, k:k + 1], scalar2=None, op0=ALU.mult),
                    reads=[("xc", k0 // 4), "ngs"], writes=["Wg"])
        for l in range(n_prev):
            wo_v = wo_d[l].rearrange("(e p) n -> p e n", p=128)
            for e4 in range(4):
                S.dma("sp", "wst", wstage[:], wo_v[:, e4, :], writes=["wstage"])
                S.op("dve", lambda e, l=l, e4=e4: e.tensor_copy(out=Wo[l][:, e4, :], in_=wstage[:]),
                     reads=["wstage"], writes=[("Wo", l)])

        S.op("pool", lambda e: e.memset(V1[:, :, 64:VW], 1.0), writes=["V1ones"])
        S.op("pool", lambda e: e.memset(V2[:, :, 64:VW], 1.0), writes=["V2ones"])
        for i in range(nq):
            S.op("pool", lambda e, i=i: e.memset(Qp[i][:], 0.0), writes=[("Qp", i)])
        if even:
            S.op("pool", lambda e: e.memset(kmT[:], 0.0), writes=["kmT"])
            S.op("pool", lambda e: e.memset(gsb[:], -1e30), writes=["gsb"])
            S.op("pool", lambda e: e.memset(biasT[:], 0.0), writes=["biasT"])
        else:
            lambda_init = 0.8 - 0.6 * math.exp(-0.3 * layer)
            S.op("pool", lambda e: e.memset(ones1[:], 1.0), writes=["ones1"])
            S.op("dve", lambda e: e.tensor_tensor(out=lam[:, 0:32], in0=lam[:, 0:32], in1=lam[:, 32:64],
                                                  op=ALU.mult),
                 reads=["lam"], writes=["lam"])
            S.op("dve", lambda e: e.tensor_tensor(out=lam[:, 64:96], in0=lam[:, 64:96], in1=lam[:, 96:128],
                                                  op=ALU.mult), reads=["lam"], writes=["lam"])
            S.op("dve", lambda e: e.tensor_reduce(out=lamw[:, 0:1], in_=lam[:, 0:32], axis=AX.X, op=ALU.add),
                 reads=["lam"], writes=["lamw"])
            S.op("dve", lambda e: e.tensor_reduce(out=lamw[:, 1:2], in_=lam[:, 64:96], axis=AX.X, op=ALU.add),
                 reads=["lam", "lamw"], writes=["lamw"])
            S.op("act", lambda e: e.activation(out=lamw[:, 2:4], in_=lamw[:, 0:2], func=AF.Exp),
                 reads=["lamw"], writes=["lamw"])
            S.op("dve", lambda e: e.tensor_tensor(out=lamw[:, 4:5], in0=lamw[:, 3:4], in1=lamw[:, 2:3],
                                                  op=ALU.subtract), reads=["lamw"], writes=["lamw"])
            S.op("dve", lambda e: e.tensor_scalar(out=lamw[:, 5:6], in0=lamw[:, 4:5], scalar1=-lambda_init,
                                                  scalar2=None, op0=ALU.add), reads=["lamw"], writes=["lamw"])
            S.op("pe", lambda e: e.matmul(B[6][0:64, 0:1], lhsT=ones1[0:1, 0:64], rhs=lamw[0:1, 5:6],
                                          start=True, stop=True), reads=["ones1", "lamw"], writes=[("B", 6)])
            S.op("dve", lambda e: e.tensor_copy(out=neglam[:], in_=B[6][0:64, 0:1]),
                 reads=[("B", 6)], writes=["neglam"])
            S.op("dve", lambda e: e.tensor_scalar(out=subgs[:], in0=subg[:], scalar1=1.0 - lambda_init,
                                                  scalar2=None, op0=ALU.mult), reads=["subg"], writes=["subgs"])

        xT_v = xT.rearrange("(k p) t -> p k t", p=128)
        yp_v = [yp_d[l].rearrange("(e p) t -> p e t", p=128) for l in range(n_prev)]

        state = {"s": 0, "p": 0, "y": 0}

        def kt_reads(kt):
            return [("KT", kt // 4)]

        def attn_softmax(c, qp_i, scale, Vt, vname, ktlist, maskfn, obank, use_bias=False):
            n = len(ktlist)
            Pl = {}
            for i in range(n + 2):
                if i < n:
                    kt = ktlist[i]
                    bi = state["s"] % 4
                    state["s"] += 1
                    pi = state["p"] % 4
                    state["p"] += 1
                    Pl[i] = pi
                    S.op("pe", lambda e, kt=kt, bi=bi: e.matmul(
                        B[bi][:, :], lhsT=KT[:, kt * 128:(kt + 1) * 128], rhs=Qp[qp_i][:],
                        start=True, stop=not use_bias),
                        reads=kt_reads(kt) + [("Qp", qp_i)], writes=[("B", bi)])
                    if use_bias:
                        S.op("pe", lambda e, kt=kt, bi=bi: e.matmul(
                            B[bi][:, :], lhsT=esel[:, kt // 2, :], rhs=biasT[:], start=False, stop=True),
                            reads=esel_keys + ["biasT"], writes=[("B", bi)])
                    S.op("act", lambda e, bi=bi, pi=pi: e.activation(
                        out=Pb[pi][:], in_=B[bi][:, :], func=AF.Exp, scale=scale),
                        reads=[("B", bi)], writes=[("Pb", pi)])
                    m = maskfn(kt)
                    if m is not None:
                        mk, map_ = m
                        S.op("dve", lambda e, pi=pi, map_=map_: e.tensor_tensor(
                            out=Pb[pi][:], in0=Pb[pi][:], in1=map_, op=ALU.mult),
                            reads=[("Pb", pi)] + mk, writes=[("Pb", pi)])
                if i >= 2:
                    j = i - 2
                    kt = ktlist[j]
                    pi = Pl[j]
                    S.op("pe", lambda e, kt=kt, pi=pi, j=j: e.matmul(
                        B[obank][0:65, :], lhsT=Vt[:, kt, 0:65], rhs=Pb[pi][:],
                        start=(j == 0), stop=(j == n - 1)),
                        reads=[(vname, kt // 4), vname + "ones", ("Pb", pi)], writes=[("B", obank)])

        def normalize(obank, oi):
            S.op("act", lambda e: e.activation(out=osb[oi][0:65, :], in_=B[obank][0:65, :], func=AF.Copy),
                 reads=[("B", obank)], writes=[("osb", oi)])
            S.op("pe", lambda e: e.matmul(B[6][0:64, :], lhsT=sel65[0:65, 0:64], rhs=osb[oi][0:65, :],
                                          start=True, stop=True),
                 reads=["sel65", ("osb", oi)], writes=[("B", 6)])
            S.op("dve", lambda e: e.reciprocal(out=rden[oi][:], in_=B[6][0:64, :]),
                 reads=[("B", 6)], writes=[("rden", oi)])
            S.op("dve", lambda e: e.tensor_tensor(out=on[oi][:], in0=osb[oi][0:64, :], in1=rden[oi][:],
                                                  op=ALU.mult),
                 reads=[("osb", oi), ("rden", oi)], writes=[("on", oi)])

        def store_y(c, m, yi):
            S.dma("sp", f"yst{yi}", y_out[m * 64:(m + 1) * 64, c * CH:(c + 1) * CH], ych[yi][:],
                  reads=[("ych", yi)])

        def rope(bank, dst, dst_key):
            S.op("dve", lambda e: e.tensor_tensor(out=tq[:], in0=B[bank][:, :], in1=rstd[:], op=ALU.mult),
                 reads=[("B", bank), "rstd"], writes=["tq"])
            S.op("pe", lambda e: e.matmul(B[5][:, :], lhsT=rm[:], rhs=tq[:], start=True, stop=True),
                 reads=["rm", "tq"], writes=[("B", 5)])
            S.op("pool", lambda e: e.tensor_tensor(out=ta[:], in0=tq[:], in1=cosc[:], op=ALU.mult),
                 reads=["tq", "cosc"], writes=["ta"])
            S.op("dve", lambda e: e.tensor_tensor(out=tb[:], in0=B[5][:, :], in1=sinc[:], op=ALU.mult),
                 reads=[("B", 5), "sinc"], writes=["tb"])
            S.op("pool", lambda e: e.tensor_tensor(out=dst[:], in0=ta[:], in1=tb[:], op=ALU.add),
                 reads=["ta", "tb"], writes=[dst_key])

        def load_chunk(cc):
            tt = cc * CH
            for q4 in range(4):
                S.dma("sp", f"xld{q4}", xc[:, 4 * q4:4 * q4 + 4, :], xT_v[:, 4 * q4:4 * q4 + 4, tt:tt + CH],
                      writes=[("xc", q4)])
            for l in range(n_prev):
                S.dma("sp", f"ypld{l}", ypc[l][:], yp_v[l][:, :, tt:tt + CH], writes=[("ypc", l)])
            S.dma("sp", "cosld", cosc[:], cos_d[:, tt:tt + CH], writes=["cosc"])
            S.dma("sp", "sinld", sinc[:], sin_d[:, tt:tt + CH], writes=["sinc"])

        for c in range(nch):
            t0 = c * CH
            if c == 0:
                load_chunk(0)
            if n_prev:
                for f in range(KC):
                    bk = 5 + (f % 2)
                    tot = n_prev * 4
                    idx = 0
                    for l in range(n_prev):
                        for e4 in range(4):
                            S.op("pe", lambda e, l=l, e4=e4, f=f, bk=bk, idx=idx: e.matmul(
                                B[bk][:, :], lhsT=Wo[l][:, e4, f * 128:(f + 1) * 128], rhs=ypc[l][:, e4, :],
                                start=(idx == 0), stop=(idx == tot - 1)),
                                reads=[("Wo", l), ("ypc", l)], writes=[("B", bk)])
                            idx += 1
                    S.op("dve", lambda e, f=f, bk=bk: e.tensor_tensor(
                        out=xc[:, f, :], in0=B[bk][:, :], in1=xc[:, f, :], op=ALU.add),
                        reads=[("B", bk), ("xc", f // 4)], writes=[("xc", f // 4)])
            for k in range(KC):
                i3 = k % 3
                S.op("act", lambda e, k=k, i3=i3: e.activation(out=sq[i3][:], in_=xc[:, k, :], func=AF.Square),
                     reads=[("xc", k // 4)], writes=[("sq", i3)])
                S.op("pool", lambda e, k=k, i3=i3: e.tensor_copy(out=xb[i3][:], in_=xc[:, k, :]),
                     reads=[("xc", k // 4)], writes=[("xb", i3)])
                S.op("pe", lambda e, k=k, i3=i3: e.matmul(B[0][:, :], lhsT=onesb[:], rhs=sq[i3][:],
                                                         start=(k == 0), stop=(k == KC - 1)),
                     reads=["onesb", ("sq", i3)], writes=[("B", 0)])
                for g in range(4):
                    S.op("pe", lambda e, k=k, i3=i3, g=g: e.matmul(
                        B[1 + g][:, :], lhsT=Wg[:, k, g * 128:(g + 1) * 128], rhs=xb[i3][:],
                        start=(k == 0), stop=(k == KC - 1)),
                        reads=["Wg", ("xb", i3)], writes=[("B", 1 + g)])
            S.op("act", lambda e: e.activation(out=ta[:], in_=B[0][:, :], func=AF.Ln, bias=EPS,
                                               scale=1.0 / D_MODEL),
                 reads=[("B", 0)], writes=["ta"])
            S.op("act", lambda e: e.activation(out=rstd[:], in_=ta[:], func=AF.Exp, scale=-0.5),
                 reads=["ta"], writes=["rstd"])
            rope(1, qr, "qr")
            if even:
                S.op("act", lambda e: e.activation(out=Qp[0][0:64, :], in_=qr[0:64, :], func=AF.Copy),
                     reads=["qr"], writes=[("Qp", 0)])
                S.op("act", lambda e: e.activation(out=Qp[1][64:128, :], in_=qr[64:128, :], func=AF.Copy),
                     reads=["qr"], writes=[("Qp", 1)])
            else:
                S.op("act", lambda e: e.activation(out=Qp[0][0:32, :], in_=qr[0:32, :], func=AF.Copy),
                     reads=["qr"], writes=[("Qp", 0)])
                S.op("act", lambda e: e.activation(out=Qp[1][32:64, :], in_=qr[32:64, :], func=AF.Copy),
                     reads=["qr"], writes=[("Qp", 1)])
                S.op("act", lambda e: e.activation(out=Qp[2][64:128, :], in_=qr[64:128, :], func=AF.Copy,
                                                   scale=0.125),
                     reads=["qr"], writes=[("Qp", 2)])
            rope(2, kr, "kr")
            S.op("act", lambda e: e.activation(out=KT[:, t0:t0 + CH], in_=kr[:], func=AF.Copy),
                 reads=["kr"], writes=[("KT", c)])
            S.op("dve", lambda e: e.tensor_tensor(out=vbs[:], in0=B[3][:, :], in1=rstd[:], op=ALU.mult),
                 reads=[("B", 3), "rstd"], writes=["vbs"])
            for j in range(4):
                S.op("pe", lambda e, j=j: e.transpose(BT[:, j, :], vbs[:, j * 128:(j + 1) * 128], identb[:]),
                     reads=["vbs", "identb"], writes=["BT"])
            S.op("act", lambda e: e.activation(out=V1[:, 4 * c:4 * c + 4, 0:64], in_=BT[:, :, 0:64], func=AF.Copy),
                 reads=["BT"], writes=[("V1", c)])
            S.op("act", lambda e: e.activation(out=V2[:, 4 * c:4 * c + 4, 0:64], in_=BT[:, :, 64:128],
                                               func=AF.Copy),
                 reads=["BT"], writes=[("V2", c)])
            S.op("dve", lambda e: e.tensor_tensor(out=tq[:], in0=B[4][:, :], in1=rstd[:], op=ALU.mult),
                 reads=[("B", 4), "rstd"], writes=["tq"])
            S.op("act", lambda e: e.activation(out=gate[:], in_=tq[:], func=AF.Silu),
                 reads=["tq"], writes=["gate"])
            S.op("dve", lambda e: e.tensor_copy(out=gate2[:], in_=gate[64:128, :]),
                 reads=["gate"], writes=["gate2"])

            if c + 1 < nch:
                load_chunk(c + 1)

            if even:
                S.op("dve", lambda e: e.tensor_reduce(
                    out=kmT[64:128, 2 * c:2 * c + 2], in_=kr[64:128, :].rearrange("p (b t) -> p b t", t=256),
                    axis=AX.X, op=ALU.add), reads=["kr"], writes=["kmT"])
                for j in range(4):
                    cur = (4 * c + j) // 2
                    if cur <= 3:
                        S.op("dve", lambda e: e.memset(biasq[:], 0.0), writes=["biasq"])
                    else:
                        S.op("pe", lambda e, j=j: e.matmul(B[6][:, 0:64], lhsT=qr[64:128, j * 128:(j + 1) * 128],
                                                          rhs=kmT[64:128, :], start=True, stop=True),
                             reads=["qr", "kmT"], writes=[("B", 6)])
                        S.op("dve", lambda e, cur=cur: e.tensor_copy(out=gsb[:, 0:cur], in_=B[6][:, 0:cur]),
                             reads=[("B", 6)], writes=["gsb"])
                        S.op("dve", lambda e: e.max(out=max8[:], in_=gsb[:]), reads=["gsb"], writes=["max8"])
                        S.op("dve", lambda e: e.tensor_scalar(out=biasq[:], in0=gsb[:], scalar1=max8[:, 2:3],
                                                              scalar2=NEG, op0=ALU.is_lt, op1=ALU.mult),
                             reads=["gsb", "max8"], writes=["biasq"])
                        S.op("dve", lambda e, cur=cur: e.memset(biasq[:, cur:cur + 1], 0.0),
                             reads=["biasq"], writes=["biasq"])
                    S.op("pe", lambda e, j=j: e.transpose(BT[0:64, j, :], biasq[:, :], identb[:]),
                         reads=["biasq", "identb"], writes=["BT"])
                S.op("dve", lambda e: e.tensor_copy(
                    out=biasT[64:128, :], in_=BT[0:64, :, :].rearrange("p a b -> p (a b)")),
                    reads=["BT"], writes=["biasT"])

                kts = [kt for kt in range(4 * c - 16, 4 * c + 4) if kt >= 0]

                def mask_a(kt, c=c):
                    o = kt - (4 * c - 16)
                    o = o if o <= 3 else (4 if o <= 11 else o - 7)
                    return (mdil_keys, mdil[:, o, :])
                attn_softmax(c, 0, 0.125, V1, "V1", kts, mask_a, 4)
                normalize(4, 0)
                yi = state["y"] % 2
                state["y"] += 1
                S.op("dve", lambda e, yi=yi: e.tensor_tensor(out=ych[yi][:], in0=on[0][:], in1=gate[0:64, :],
                                                             op=ALU.mult),
                     reads=[("on", 0), "gate"], writes=[("ych", yi)])
                store_y(c, 0, yi)

                kts = list(range(0, 4 * c + 4))

                def mask_b(kt, c=c):
                    i = kt - 4 * c
                    if i < 0:
                        return None
                    return (["mincl"], mincl[:, i, :])
                attn_softmax(c, 1, 0.125, V2, "V2", kts, mask_b, 5, use_bias=True)
                normalize(5, 1)
                yi = state["y"] % 2
                state["y"] += 1
                S.op("dve", lambda e, yi=yi: e.tensor_tensor(out=ych[yi][:], in0=on[1][:], in1=gate2[:],
                                                             op=ALU.mult),
                     reads=[("on", 1), "gate2"], writes=[("ych", yi)])
                store_y(c, 1, yi)
            else:
                kts = list(range(0, 4 * c + 4))

                def mask_c(kt, c=c):
                    i = kt - 4 * c
                    if i < 0:
                        return None
                    return (["mincl"], mincl[:, i, :])
                sc = 1.0 / math.sqrt(32.0)
                attn_softmax(c, 0, sc, V1, "V1", kts, mask_c, 4)
                attn_softmax(c, 1, sc, V1, "V1", kts, mask_c, 5)
                normalize(4, 0)
                normalize(5, 1)
                S.op("dve", lambda e: e.scalar_tensor_tensor(out=on[0][:], in0=on[1][:], scalar=neglam[:, 0:1],
                                                             in1=on[0][:], op0=ALU.mult, op1=ALU.add),
                     reads=[("on", 0), ("on", 1), "neglam"], writes=[("on", 0)])
                S.op("act", lambda e: e.activation(out=on[1][:], in_=on[0][:], func=AF.Square),
                     reads=[("on", 0)], writes=[("on", 1)])
                S.op("pe", lambda e: e.matmul(B[6][0:64, :], lhsT=ones64[:], rhs=on[1][:], start=True, stop=True),
                     reads=["ones64", ("on", 1)], writes=[("B", 6)])
                S.op("act", lambda e: e.activation(out=rden[0][:], in_=B[6][0:64, :], func=AF.Ln, bias=EPS,
                                                   scale=1.0 / 64.0),
                     reads=[("B", 6)], writes=[("rden", 0)])
                S.op("act", lambda e: e.activation(out=rden[1][:], in_=rden[0][:], func=AF.Exp, scale=-0.5),
                     reads=[("rden", 0)], writes=[("rden", 1)])
                S.op("dve", lambda e: e.tensor_tensor(out=on[0][:], in0=on[0][:], in1=rden[1][:], op=ALU.mult),
                     reads=[("on", 0), ("rden", 1)], writes=[("on", 0)])
                yi = state["y"] % 2
                state["y"] += 1
                S.op("dve", lambda e, yi=yi: e.scalar_tensor_tensor(
                    out=ych[yi][:], in0=on[0][:], scalar=subgs[:, 0:1], in1=gate[0:64, :],
                    op0=ALU.mult, op1=ALU.mult),
                    reads=[("on", 0), "subgs", "gate"], writes=[("ych", yi)])
                store_y(c, 0, yi)

                kts = list(range(4 * c + 3, -1, -1))
                n = len(kts)
                info = {}
                for i in range(n + 2):
                    if i < n:
                        kt = kts[i]
                        sbk = i % 2
                        ei = i % 2
                        si = i % 3
                        info[i] = (kt, si)
                        S.op("pe", lambda e, kt=kt, sbk=sbk: e.matmul(
                            B[sbk][:, :], lhsT=KT[:, kt * 128:(kt + 1) * 128], rhs=Qp[2][:], start=True, stop=True),
                            reads=kt_reads(kt) + [("Qp", 2)], writes=[("B", sbk)])
                        S.op("act", lambda e, sbk=sbk, ei=ei: e.activation(out=e1[ei][:], in_=B[sbk][:, :],
                                                                         func=AF.Exp),
                             reads=[("B", sbk)], writes=[("e1", ei)])
                        cur_i = (kt, ei, si)
                    if 1 <= i <= n:
                        j = i - 1
                        kt, si = info[j]
                        lb = 2 + (j % 2)
                        S.op("pe", lambda e, si=si, lb=lb: e.matmul(B[lb][:, :], lhsT=ntri[:], rhs=spb[si][:],
                                                                    start=True, stop=False),
                             reads=["ntri", ("spb", si)], writes=[("B", lb)])
                        if j > 0:
                            S.op("pe", lambda e, lb=lb, j=j: e.matmul(B[lb][:, :], lhsT=nones[:],
                                                                      rhs=spsum[(j - 1) % 2][:],
                                                                      start=False, stop=False),
                                 reads=["nones", ("spsum", (j - 1) % 2)], writes=[("B", lb)])
                        S.op("pe", lambda e, kt=kt, lb=lb: e.matmul(
                            B[lb][:, :], lhsT=KT[:, kt * 128:(kt + 1) * 128], rhs=Qp[2][:], start=False, stop=True),
                            reads=kt_reads(kt) + [("Qp", 2)], writes=[("B", lb)])
                        if j == 0:
                            S.op("pool", lambda e, si=si: e.tensor_copy(out=spsum[0][:], in_=spb[si][:]),
                                 reads=[("spb", si)], writes=[("spsum", 0)])
                        elif j < n - 1:
                            S.op("pool", lambda e, si=si, j=j: e.tensor_tensor(
                                out=spsum[j % 2][:], in0=spsum[(j - 1) % 2][:], in1=spb[si][:], op=ALU.add),
                                reads=[("spsum", (j - 1) % 2), ("spb", si)], writes=[("spsum", j % 2)])
                        pi = state["p"] % 4
                        state["p"] += 1
                        info[j] = (kt, si, pi)
                        S.op("act", lambda e, lb=lb, pi=pi: e.activation(out=Pb[pi][:], in_=B[lb][:, :], func=AF.Exp),
                             reads=[("B", lb)], writes=[("Pb", pi)])
                        di = kt - 4 * c
                        if di >= 0:
                            S.op("dve", lambda e, pi=pi, di=di: e.tensor_tensor(
                                out=Pb[pi][:], in0=Pb[pi][:], in1=mstrict[:, di, :], op=ALU.mult),
                                reads=[("Pb", pi), "mstrict"], writes=[("Pb", pi)])
                    if i < n:
                        kt_i, ei_i, si_i = cur_i
                        S.op("act", lambda e, ei=ei_i, si=si_i: e.activation(out=spb[si][:], in_=e1[ei][:],
                                                                           func=AF.Ln, bias=1.0),
                             reads=[("e1", ei_i)], writes=[("spb", si_i)])
                        di = kt_i - 4 * c
                        if di >= 0:
                            S.op("dve", lambda e, si=si_i, di=di: e.tensor_tensor(
                                out=spb[si][:], in0=spb[si][:], in1=mstrict[:, di, :], op=ALU.mult),
                                reads=[("spb", si_i), "mstrict"], writes=[("spb", si_i)])
                    if i >= 2:
                        j = i - 2
                        kt, si, pi = info[j]
                        S.op("pe", lambda e, kt=kt, pi=pi, j=j: e.matmul(
                            B[4][0:65, :], lhsT=V2[:, kt, 0:65], rhs=Pb[pi][:], start=(j == 0), stop=(j == n - 1)),
                            reads=[("V2", kt // 4), "V2ones", ("Pb", pi)], writes=[("B", 4)])
                yi = state["y"] % 2
                state["y"] += 1
                S.op("dve", lambda e, yi=yi: e.tensor_tensor(out=ych[yi][:], in0=B[4][0:64, :], in1=gate2[:],
                                                             op=ALU.mult),
                     reads=[("B", 4), "gate2"], writes=[("ych", yi)])
                store_y(c, 1, yi)

        S.finish("sp")
        nc._n_inst = S.n_inst
    return nc


def build_final(n_prev, ntok, do_norm=True):
    nch = ntok // CH
    nc = bass.Bass("TRN2", target_bir_lowering=False)

    def din(name, shape, dt):
        return nc.dram_tensor(name, shape, dt, kind="ExternalInput").ap()

    xT = din("xT", [D_MODEL, ntok], F32)
    fg_d = din("fg", [128, KC], F32)
    onesb_d = din("onesb", [128, 128], BF16)
    yp_d = [din(f"yp{l}", [512, ntok], BF16) for l in range(n_prev)]
    wo_d = [din(f"wo{l}", [512, D_MODEL], F32) for l in range(n_prev)]
    o_out = nc.dram_tensor("o", [D_MODEL, ntok], F32, kind="ExternalOutput").ap()

    with contextlib.ExitStack() as st:
        def sb(name, shape, dt):
            return st.enter_context(nc.sbuf_tensor(name, shape, dt))

        def ps(name, shape, dt):
            return st.enter_context(nc.psum_tensor(name, shape, dt))

        S = Sched(nc, st)
        Wo = [sb(f"Wo{l}", [128, 4, D_MODEL], BF16) for l in range(n_prev)]
        wstage = sb("wstage", [128, 2048], F32)
        fg = sb("fg_s", [128, KC], F32)
        onesb = sb("onesb_s", [128, 128], BF16)
        xc = [sb(f"xc{i}", [128, KC, CH], F32) for i in range(2)]
        ypc = [[sb(f"ypc{l}_{i}", [128, 4, CH], BF16) for l in range(n_prev)] for i in range(2)]
        sq = [sb(f"sq{i}", [128, CH], BF16) for i in range(3)]
        lnt = sb("lnt", [128, CH], F32)
        rstd = sb("rstd", [128, CH], F32)
        B = [ps(f"B{i}", [128, 512], F32) for i in range(4)]

        S.dma("sp", "c0", fg[:], fg_d[:, :], writes=["fg"])
        S.dma("sp", "c1", onesb[:], onesb_d[:, :], writes=["onesb"])
        for l in range(n_prev):
            wo_v = wo_d[l].rearrange("(e p) n -> p e n", p=128)
            for e4 in range(4):
                S.dma("sp", "wst", wstage[:], wo_v[:, e4, :], writes=["wstage"])
                S.op("dve", lambda e, l=l, e4=e4: e.tensor_copy(out=Wo[l][:, e4, :], in_=wstage[:]),
                     reads=["wstage"], writes=[("Wo", l)])
        xT_v = xT.rearrange("(k p) t -> p k t", p=128)
        o_v = o_out.rearrange("(k p) t -> p k t", p=128)
        yp_v = [yp_d[l].rearrange("(e p) t -> p e t", p=128) for l in range(n_prev)]
        for c in range(nch):
            t0 = c * CH
            xi = c % 2
            for q4 in range(4):
                S.dma("sp", f"xld{xi}_{q4}", xc[xi][:, 4 * q4:4 * q4 + 4, :],
                      xT_v[:, 4 * q4:4 * q4 + 4, t0:t0 + CH], writes=[("xc", xi, q4)])
            for l in range(n_prev):
                S.dma("sp", f"ypld{l}_{xi}", ypc[xi][l][:], yp_v[l][:, :, t0:t0 + CH], writes=[("ypc", xi, l)])
            for f in range(KC):
                bk = 1 + (f % 2)
                tot = n_prev * 4
                idx = 0
                for l in range(n_prev):
                    for e4 in range(4):
                        S.op("pe", lambda e, l=l, e4=e4, f=f, bk=bk, idx=idx: e.matmul(
                            B[bk][:, :], lhsT=Wo[l][:, e4, f * 128:(f + 1) * 128], rhs=ypc[xi][l][:, e4, :],
                            start=(idx == 0), stop=(idx == tot - 1)),
                            reads=[("Wo", l), ("ypc", xi, l)], writes=[("B", bk)])
                        idx += 1
                S.op("dve", lambda e, f=f, bk=bk: e.tensor_tensor(
                    out=xc[xi][:, f, :], in0=B[bk][:, :], in1=xc[xi][:, f, :], op=ALU.add),
                    reads=[("B", bk), ("xc", xi, f // 4)], writes=[("xc", xi, f // 4)])
            for k in range(KC if do_norm else 0):
                i3 = k % 3
                S.op("act", lambda e, k=k, i3=i3: e.activation(out=sq[i3][:], in_=xc[xi][:, k, :], func=AF.Square),
                     reads=[("xc", xi, k // 4)], writes=[("sq", i3)])
                S.op("pe", lambda e, k=k, i3=i3: e.matmul(B[0][:, :], lhsT=onesb[:], rhs=sq[i3][:],
                                                         start=(k == 0), stop=(k == KC - 1)),
                     reads=["onesb", ("sq", i3)], writes=[("B", 0)])
            if do_norm:
                S.op("act", lambda e: e.activation(out=lnt[:], in_=B[0][:, :], func=AF.Ln, bias=EPS,
                                                   scale=1.0 / D_MODEL),
                     reads=[("B", 0)], writes=["lnt"])
                S.op("act", lambda e: e.activation(out=rstd[:], in_=lnt[:], func=AF.Exp, scale=-0.5),
                     reads=["lnt"], writes=["rstd"])
            for k in range(KC if do_norm else 0):
                eng = "dve"
                S.op(eng, lambda e, k=k: e.scalar_tensor_tensor(
                    out=xc[xi][:, k, :], in0=xc[xi][:, k, :], scalar=fg[:, k:k + 1], in1=rstd[:],
                    op0=ALU.mult, op1=ALU.mult),
                    reads=[("xc", xi, k // 4), "fg", "rstd"], writes=[("xc", xi, k // 4)])
            for q4 in range(4):
                S.dma("sp", f"ost{xi}_{q4}", o_v[:, 4 * q4:4 * q4 + 4, t0:t0 + CH],
                      xc[xi][:, 4 * q4:4 * q4 + 4, :], reads=[("xc", xi, q4)])
        S.finish("sp")
    return nc


_PROG = {}
_DEBUG_DIR = None


def _prog(key, fn):
    if key not in _PROG:
        _PROG[key] = fn()
    return _PROG[key]


def _w_cols(h):
    cols = []
    for part in range(4):
        for mixer in range(2):
            base = (mixer * 4 + part) * 256 + h * 64
            cols.extend(range(base, base + 64))
    return np.asarray(cols)


def kernel(x, norm_g, w_in, w_out, diff_lam, diff_subln_g, final_norm_g):
    x = np.asarray(x, np.float32)
    norm_g = np.asarray(norm_g, np.float32)
    w_in = np.asarray(w_in, np.float32)
    w_out = np.asarray(w_out, np.float32)
    diff_lam = np.asarray(diff_lam, np.float32)
    diff_subln_g = np.asarray(diff_subln_g, np.float32)
    final_norm_g = np.asarray(final_norm_g, np.float32)
    nb, seq, _ = x.shape
    ntok = seq // 4
    xTs = [np.ascontiguousarray(x[b].T) for b in range(nb)]
    fg = np.ascontiguousarray(final_norm_g.reshape(KC, 128).T)
    onesb = np.ones((128, 128), np.float32).astype(NPBF)
    out = None
    for layer in range(DEPTH):
        even = layer % 2 == 0
        nc = _prog(("layer", layer, seq), lambda: build_layer(layer, 0, seq))
        cos, sin, rm = _rope_tables(even, seq)
        consts = _consts(even)
        ng = np.ascontiguousarray(norm_g[layer].reshape(KC, 128).T)
        in_maps = []
        for core in range(8):
            b, h = core // 4, core % 4
            m = {"xT": xTs[b], "w": np.ascontiguousarray(w_in[layer][:, _w_cols(h)]), "ng": ng,
                 "cos": cos, "sin": sin, "rm": rm}
            m.update(consts)
            if not even:
                m["lam"] = np.ascontiguousarray(diff_lam[layer // 2].reshape(1, 128))
                m["subg"] = np.ascontiguousarray(diff_subln_g[layer // 2].reshape(64, 1))
            in_maps.append(m)
        res = run_bass_kernel_spmd(nc, in_maps, core_ids=list(range(8)))
        ylay = []
        for b in range(nb):
            yb = np.empty((512, seq), NPBF)
            for h in range(4):
                r = np.asarray(res.results[b * 4 + h]["y"])
                yb[h * 64:(h + 1) * 64] = r[0:64]
                yb[256 + h * 64:256 + (h + 1) * 64] = r[64:128]
            ylay.append(yb)
        if _DEBUG_DIR:
            np.save(f"{_DEBUG_DIR}/dbg_y{layer}.npy", np.stack(ylay).view(np.uint16))
        last = layer == DEPTH - 1
        ncu = _prog(("upd", ntok, last), lambda: build_final(1, ntok, do_norm=last))
        in_maps = []
        for core in range(8):
            b, j = core // 4, core % 4
            sl = slice(j * ntok, (j + 1) * ntok)
            in_maps.append({"xT": np.ascontiguousarray(xTs[b][:, sl]), "fg": fg, "onesb": onesb,
                            "yp0": np.ascontiguousarray(ylay[b][:, sl]), "wo0": w_out[layer]})
        res = run_bass_kernel_spmd(ncu, in_maps, core_ids=list(range(8)))
        if not last:
            for core in range(8):
                b, j = core // 4, core % 4
                xTs[b][:, j * ntok:(j + 1) * ntok] = np.asarray(res.results[core]["o"])
        else:
            out = np.empty((nb, seq, D_MODEL), np.float32)
            for core in range(8):
                b, j = core // 4, core % 4
                out[b, j * ntok:(j + 1) * ntok, :] = np.asarray(res.results[core]["o"]).T
    return out
```

```python
import contextlib
import math

import ml_dtypes
import numpy as np

import concourse.bass as bass
import concourse.mybir as mybir
from concourse.bass_utils import run_bass_kernel_spmd

F32 = mybir.dt.float32
BF16 = mybir.dt.bfloat16
AF = mybir.ActivationFunctionType
ALU = mybir.AluOpType
AX = mybir.AxisListType
NPBF = ml_dtypes.bfloat16

D_MODEL = 2048
SEQ = 16384
DEPTH = 4
CH = 512
KC = 16
EPS = 1e-6
VW = 72
NEG = -30000.0


class Sched:
    SEG = 16000

    def __init__(self, nc, stack):
        self.nc = nc
        self.stack = stack
        self.eng = {"pe": nc.tensor, "act": nc.scalar, "dve": nc.vector, "pool": nc.gpsimd, "sp": nc.sync}
        self.cnt = {e: 0 for e in self.eng}
        self.sems = {e: [] for e in self.eng}
        self.waited = {e: {} for e in self.eng}
        self.lastw = {}
        self.reads = {}
        self.dsem = {}
        self.dcnt = {}
        self.n_inst = 0

    def _sem(self, e, seg):
        while len(self.sems[e]) <= seg:
            self.sems[e].append(self.stack.enter_context(self.nc.semaphore(f"s_{e}_{len(self.sems[e])}")))
        return self.sems[e][seg]

    def _wait(self, e, prod, count):
        if count <= 0:
            return
        w = self.waited[e]
        if w.get(prod, 0) >= count:
            return
        w[prod] = count
        eng = self.eng[e]
        if prod.startswith("dma:"):
            eng.wait_ge(self.dsem[prod], count)
        else:
            seg = (count - 1) // self.SEG
            eng.wait_ge(self._sem(prod, seg), (count - 1) % self.SEG + 1)

    def _deps(self, e, reads, writes):
        for r in reads:
            lw = self.lastw.get(r)
            if lw is not None and not (lw[0] == e and e == "pe"):
                self._wait(e, lw[0], lw[1])
        for wbuf in writes:
            lw = self.lastw.get(wbuf)
            if lw is not None and lw[0] != e:
                self._wait(e, lw[0], lw[1])
            for prod, cnt in self.reads.get(wbuf, {}).items():
                if prod != e:
                    self._wait(e, prod, cnt)

    def op(self, e, fn, reads=(), writes=()):
        self._deps(e, reads, writes)
        inst = fn(self.eng[e])
        self.cnt[e] += 1
        n = self.cnt[e]
        seg = (n - 1) // self.SEG
        inst.then_inc(self._sem(e, seg), 1)
        self.n_inst += 1
        for r in reads:
            self.reads.setdefault(r, {})[e] = n
        for wbuf in writes:
            self.lastw[wbuf] = (e, n)
            self.reads[wbuf] = {}

    def dma(self, q, semname, out, in_, reads=(), writes=()):
        prod = "dma:" + semname
        if prod not in self.dsem:
            self.dsem[prod] = self.stack.enter_context(self.nc.semaphore("d_" + semname))
            self.dcnt[prod] = 0
        self._deps(q, reads, writes)
        self.eng[q].dma_start(out=out, in_=in_).then_inc(self.dsem[prod], 16)
        self.dcnt[prod] += 16
        n = self.dcnt[prod]
        self.n_inst += 1
        for r in reads:
            self.reads.setdefault(r, {})[prod] = n
        for wbuf in writes:
            self.lastw[wbuf] = (prod, n)
            self.reads[wbuf] = {}

    def finish(self, q="sp"):
        for prod, n in self.dcnt.items():
            self._wait(q, prod, n)
        for e in self.eng:
            if e != q:
                self._wait(q, e, self.cnt[e])


def _rope_tables(even, seq):
    pos = np.arange(seq, dtype=np.float32)
    cos = np.ones((128, seq), np.float32)
    sin = np.zeros((128, seq), np.float32)
    rm = np.zeros((128, 128), np.float32)
    for p in range(128):
        if even:
            base, i, hd = (p // 64) * 64, p % 64, 64
        elif p < 64:
            base, i, hd = (p // 32) * 32, p % 32, 32
        else:
            rm[p, p] = 1.0
            continue
        half = hd // 2
        inv = (np.float32(10000.0) ** (-(np.arange(half, dtype=np.float32) / np.float32(half)))).astype(np.float32)
        ang = (pos * inv[i % half]).astype(np.float32)
        cos[p] = np.cos(ang).astype(np.float32)
        sgn = -1.0 if i < half else 1.0
        sin[p] = (sgn * np.sin(ang)).astype(np.float32)
        partner = base + (i + half) % hd
        rm[partner, p] = 1.0
    return cos, sin, rm


def _consts(even):
    ki = np.arange(128)[:, None]
    qi = np.arange(512)[None, :]
    c = {}
    incl = np.stack([((128 * i + ki) <= qi) for i in range(4)], 1).astype(np.float32)
    strict = np.stack([((128 * i + ki) < qi) for i in range(4)], 1).astype(np.float32)
    c["mincl"] = incl.astype(NPBF)
    c["mstrict"] = strict.astype(NPBF)
    if even:
        dm = []
        for o in range(20):
            d = 2048 - 128 * o + qi - ki
            m = ((d >= 0) & (d <= 128)).astype(np.float32)
            m += ((d >= 0) & (d % 4 == 0) & (d <= 512)).astype(np.float32)
            m += ((d >= 0) & (d % 16 == 0) & (d <= 2048)).astype(np.float32)
            dm.append(m)
        assert all(np.array_equal(dm[4], dm[o]) for o in range(4, 12))
        dm = dm[0:5] + dm[12:20]
        c["mdil"] = np.stack(dm, 1).astype(NPBF)
        e = np.zeros((128, 64, 128), np.float32)
        for blk in range(64):
            e[64 + blk, blk, :] = 1.0
        c["esel"] = e.astype(NPBF)
    else:
        j = np.arange(128)[:, None]
        s = np.arange(128)[None, :]
        c["ntri"] = (-(j >= s).astype(np.float32)).astype(NPBF)
        c["nones"] = (-np.ones((128, 128), np.float32)).astype(NPBF)
    c["onesb"] = np.ones((128, 128), np.float32).astype(NPBF)
    c["identb"] = np.eye(128, dtype=np.float32).astype(NPBF)
    sel = np.zeros((128, 64), np.float32)
    sel[64, :] = 1.0
    c["sel65"] = sel
    c["ones64"] = np.ones((64, 64), np.float32)
    return c


def build_layer(layer, n_prev, seq=SEQ):
    even = layer % 2 == 0
    nch = seq // CH
    ntile = seq // 128
    nc = bass.Bass("TRN2", target_bir_lowering=False)

    def din(name, shape, dt):
        return nc.dram_tensor(name, shape, dt, kind="ExternalInput").ap()

    xT = din("xT", [D_MODEL, seq], F32)
    w_d = din("w", [D_MODEL, 512], F32)
    ng_d = din("ng", [128, KC], F32)
    cos_d = din("cos", [128, seq], F32)
    sin_d = din("sin", [128, seq], F32)
    rm_d = din("rm", [128, 128], F32)
    mincl_d = din("mincl", [128, 4, 512], BF16)
    mstrict_d = din("mstrict", [128, 4, 512], BF16)
    onesb_d = din("onesb", [128, 128], BF16)
    identb_d = din("identb", [128, 128], BF16)
    sel65_d = din("sel65", [128, 64], F32)
    ones64_d = din("ones64", [64, 64], F32)
    if even:
        mdil_d = din("mdil", [128, 13, 512], BF16)
        esel_d = din("esel", [128, 64, 128], BF16)
    else:
        ntri_d = din("ntri", [128, 128], BF16)
        nones_d = din("nones", [128, 128], BF16)
        lam_d = din("lam", [1, 128], F32)
        subg_d = din("subg", [64, 1], F32)
    yp_d = [din(f"yp{l}", [512, seq], BF16) for l in range(n_prev)]
    wo_d = [din(f"wo{l}", [512, D_MODEL], F32) for l in range(n_prev)]
    y_out = nc.dram_tensor("y", [128, seq], BF16, kind="ExternalOutput").ap()

    with contextlib.ExitStack() as st:
        def sb(name, shape, dt):
            return st.enter_context(nc.sbuf_tensor(name, shape, dt))

        def ps(name, shape, dt):
            return st.enter_context(nc.psum_tensor(name, shape, dt))

        S = Sched(nc, st)

        Wg = sb("Wg", [128, KC, 512], BF16)
        Wo = [sb(f"Wo{l}", [128, 4, D_MODEL], BF16) for l in range(n_prev)]
        KT = sb("KT", [128, seq], BF16)
        V1 = sb("V1", [128, ntile, VW], BF16)
        V2 = sb("V2", [128, ntile, VW], BF16)
        xc = sb("xc", [128, KC, CH], F32)
        wstage = sb("wstage", [128, 2048], F32) if n_prev else None
        ngs = sb("ngs", [128, KC], F32)
        rm = sb("rm_s", [128, 128], F32)
        mincl = sb("mincl_s", [128, 4, 512], BF16)
        mstrict = sb("mstrict_s", [128, 4, 512], BF16) if not even else None
        onesb = sb("onesb_s", [128, 128], BF16)
        identb = sb("identb_s", [128, 128], BF16)
        sel65 = sb("sel65_s", [128, 64], F32)
        ones64 = sb("ones64_s", [64, 64], F32)
        if even:
            mdil = sb("mdil_s", [128, 13, 512], BF16)
            esel = sb("esel_s", [128, 64, 128], BF16)
            kmT = sb("kmT", [128, 64], F32)
            gsb = sb("gsb", [128, 64], F32)
            biasq = sb("biasq", [128, 64], BF16)
            biasT = sb("biasT", [128, 512], BF16)
            max8 = sb("max8", [128, 8], F32)
        else:
            ntri = sb("ntri_s", [128, 128], BF16)
            nones = sb("nones_s", [128, 128], BF16)
            lam = sb("lam_s", [1, 128], F32)
            lamw = sb("lamw", [1, 8], F32)
            neglam = sb("neglam", [64, 1], F32)
            subg = sb("subg_s", [64, 1], F32)
            subgs = sb("subgs", [64, 1], F32)
            ones1 = sb("ones1", [1, 64], F32)
            e1 = [sb(f"e1_{i}", [128, 512], F32) for i in range(2)]
            spb = [sb(f"spb_{i}", [128, 512], BF16) for i in range(3)]
            spsum = [sb(f"spsum_{i}", [128, 512], BF16) for i in range(2)]
        ypc = [sb(f"ypc{l}", [128, 4, CH], BF16) for l in range(n_prev)]
        sq = [sb(f"sq{i}", [128, CH], BF16) for i in range(3)]
        xb = [sb(f"xb{i}", [128, CH], BF16) for i in range(3)]
        cosc = sb("cosc", [128, CH], F32)
        sinc = sb("sinc", [128, CH], F32)
        rstd = sb("rstd", [128, CH], F32)
        tq = sb("tq", [128, CH], F32)
        ta = sb("ta", [128, CH], F32)
        tb = sb("tb", [128, CH], F32)
        qr = sb("qr", [128, CH], F32)
        kr = sb("kr", [128, CH], F32)
        vbs = sb("vbs", [128, CH], BF16)
        gate = sb("gate", [128, CH], BF16)
        gate2 = sb("gate2", [64, CH], BF16)
        nq = 2 if even else 3
        Qp = [sb(f"Qp{i}", [128, CH], BF16) for i in range(nq)]
        Pb = [sb(f"Pb{i}", [128, CH], BF16) for i in range(4)]
        osb = [sb(f"osb{i}", [128, CH], F32) for i in range(2)]
        rden = [sb(f"rden{i}", [64, CH], F32) for i in range(2)]
        on = [sb(f"on{i}", [64, CH], F32) for i in range(2)]
        ych = [sb(f"ych{i}", [64, CH], BF16) for i in range(2)]

        B = [ps(f"B{i}", [128, 512], F32) for i in range(7)]
        BT = ps("BT", [128, 4, 128], BF16)

        def load(semname, dst, dst_key, src):
            S.dma("sp", semname, dst, src, writes=[dst_key])

        load("c0", rm[:], "rm", rm_d[:, :])
        load("c1", mincl[:], "mincl", mincl_d[:, :, :])
        if not even:
            load("c2", mstrict[:], "mstrict", mstrict_d[:, :, :])
        load("c3", onesb[:], "onesb", onesb_d[:, :])
        load("c4", identb[:], "identb", identb_d[:, :])
        load("c5", sel65[:], "sel65", sel65_d[:, :])
        load("c6", ones64[:], "ones64", ones64_d[:, :])
        load("c7", ngs[:], "ngs", ng_d[:, :])
        if even:
            for o, o2 in ((0, 5), (5, 9), (9, 13)):
                load(f"c8_{o}", mdil[:, o:o2, :], ("mdil", o), mdil_d[:, o:o2, :])
            for o in range(0, 64, 16):
                load(f"c9_{o}", esel[:, o:o + 16, :], ("esel", o), esel_d[:, o:o + 16, :])
        else:
            load("c8", ntri[:], "ntri", ntri_d[:, :])
            load("c9", nones[:], "nones", nones_d[:, :])
            load("c10", lam[:], "lam", lam_d[:, :])
            load("c11", subg[:], "subg", subg_d[:, :])
        mdil_keys = [("mdil", o) for o in (0, 5, 9)]
        esel_keys = [("esel", o) for o in range(0, 64, 16)]

        w_v = w_d.rearrange("(k p) n -> p k n", p=128)
        for k0 in range(0, KC, 4):
            S.dma("sp", f"wst{k0}", xc[:, k0:k0 + 4, :], w_v[:, k0:k0 + 4, :], writes=[("xc", k0 // 4)])
            for k in range(k0, k0 + 4):
                S.op("dve", lambda e, k=k: e.tensor_scalar(
                    out=Wg[:, k, :], in0=xc[:, k, :],
                    scalar1=ngs[:, k:k + 1], scalar2=None, op0=ALU.mult),
                    reads=[("xc", k0 // 4), "ngs"], writes=["Wg"])
        for l in range(n_prev):
            wo_v = wo_d[l].rearrange("(e p) n -> p e n", p=128)
            for e4 in range(4):
                S.dma("sp", "wst", wstage[:], wo_v[:, e4, :], writes=["wstage"])
                S.op("dve", lambda e, l=l, e4=e4: e.tensor_copy(out=Wo[l][:, e4, :], in_=wstage[:]),
                     reads=["wstage"], writes=[("Wo", l)])

        S.op("pool", lambda e: e.memset(V1[:, :, 64:VW], 1.0), writes=["V1ones"])
        S.op("pool", lambda e: e.memset(V2[:, :, 64:VW], 1.0), writes=["V2ones"])
        for i in range(nq):
            S.op("pool", lambda e, i=i: e.memset(Qp[i][:], 0.0), writes=[("Qp", i)])
        if even:
            S.op("pool", lambda e: e.memset(kmT[:], 0.0), writes=["kmT"])
            S.op("pool", lambda e: e.memset(gsb[:], -1e30), writes=["gsb"])
            S.op("pool", lambda e: e.memset(biasT[:], 0.0), writes=["biasT"])
        else:
            lambda_init = 0.8 - 0.6 * math.exp(-0.3 * layer)
            S.op("pool", lambda e: e.memset(ones1[:], 1.0), writes=["ones1"])
            S.op("dve", lambda e: e.tensor_tensor(out=lam[:, 0:32], in0=lam[:, 0:32], in1=lam[:, 32:64],
                                                  op=ALU.mult),
                 reads=["lam"], writes=["lam"])
            S.op("dve", lambda e: e.tensor_tensor(out=lam[:, 64:96], in0=lam[:, 64:96], in1=lam[:, 96:128],
                                                  op=ALU.mult), reads=["lam"], writes=["lam"])
            S.op("dve", lambda e: e.tensor_reduce(out=lamw[:, 0:1], in_=lam[:, 0:32], axis=AX.X, op=ALU.add),
                 reads=["lam"], writes=["lamw"])
            S.op("dve", lambda e: e.tensor_reduce(out=lamw[:, 1:2], in_=lam[:, 64:96], axis=AX.X, op=ALU.add),
                 reads=["lam", "lamw"], writes=["lamw"])
            S.op("act", lambda e: e.activation(out=lamw[:, 2:4], in_=lamw[:, 0:2], func=AF.Exp),
                 reads=["lamw"], writes=["lamw"])
            S.op("dve", lambda e: e.tensor_tensor(out=lamw[:, 4:5], in0=lamw[:, 3:4], in1=lamw[:, 2:3],
                                                  op=ALU.subtract), reads=["lamw"], writes=["lamw"])
            S.op("dve", lambda e: e.tensor_scalar(out=lamw[:, 5:6], in0=lamw[:, 4:5], scalar1=-lambda_init,
                                                  scalar2=None, op0=ALU.add), reads=["lamw"], writes=["lamw"])
            S.op("pe", lambda e: e.matmul(B[6][0:64, 0:1], lhsT=ones1[0:1, 0:64], rhs=lamw[0:1, 5:6],
                                          start=True, stop=True), reads=["ones1", "lamw"], writes=[("B", 6)])
            S.op("dve", lambda e: e.tensor_copy(out=neglam[:], in_=B[6][0:64, 0:1]),
                 reads=[("B", 6)], writes=["neglam"])
            S.op("dve", lambda e: e.tensor_scalar(out=subgs[:], in0=subg[:], scalar1=1.0 - lambda_init,
                                                  scalar2=None, op0=ALU.mult), reads=["subg"], writes=["subgs"])

        xT_v = xT.rearrange("(k p) t -> p k t", p=128)
        yp_v = [yp_d[l].rearrange("(e p) t -> p e t", p=128) for l in range(n_prev)]

        state = {"s": 0, "p": 0, "y": 0}

        def kt_reads(kt):
            return [("KT", kt // 4)]

        def attn_softmax(c, qp_i, scale, Vt, vname, ktlist, maskfn, obank, use_bias=False):
            n = len(ktlist)
            Pl = {}
            for i in range(n + 2):
                if i < n:
                    kt = ktlist[i]
                    bi = state["s"] % 4
                    state["s"] += 1
                    pi = state["p"] % 4
                    state["p"] += 1
                    Pl[i] = pi
                    S.op("pe", lambda e, kt=kt, bi=bi: e.matmul(
                        B[bi][:, :], lhsT=KT[:, kt * 128:(kt + 1) * 128], rhs=Qp[qp_i][:],
                        start=True, stop=not use_bias),
                        reads=kt_reads(kt) + [("Qp", qp_i)], writes=[("B", bi)])
                    if use_bias:
                        S.op("pe", lambda e, kt=kt, bi=bi: e.matmul(
                            B[bi][:, :], lhsT=esel[:, kt // 2, :], rhs=biasT[:], start=False, stop=True),
                            reads=esel_keys + ["biasT"], writes=[("B", bi)])
                    S.op("act", lambda e, bi=bi, pi=pi: e.activation(
                        out=Pb[pi][:], in_=B[bi][:, :], func=AF.Exp, scale=scale),
                        reads=[("B", bi)], writes=[("Pb", pi)])
                    m = maskfn(kt)
                    if m is not None:
                        mk, map_ = m
                        S.op("dve", lambda e, pi=pi, map_=map_: e.tensor_tensor(
                            out=Pb[pi][:], in0=Pb[pi][:], in1=map_, op=ALU.mult),
                            reads=[("Pb", pi)] + mk, writes=[("Pb", pi)])
                if i >= 2:
                    j = i - 2
                    kt = ktlist[j]
                    pi = Pl[j]
                    S.op("pe", lambda e, kt=kt, pi=pi, j=j: e.matmul(
                        B[obank][0:65, :], lhsT=Vt[:, kt, 0:65], rhs=Pb[pi][:],
                        start=(j == 0), stop=(j == n - 1)),
                        reads=[(vname, kt // 4), vname + "ones", ("Pb", pi)], writes=[("B", obank)])

        def normalize(obank, oi):
            S.op("act", lambda e: e.activation(out=osb[oi][0:65, :], in_=B[obank][0:65, :], func=AF.Copy),
                 reads=[("B", obank)], writes=[("osb", oi)])
            S.op("pe", lambda e: e.matmul(B[6][0:64, :], lhsT=sel65[0:65, 0:64], rhs=osb[oi][0:65, :],
                                          start=True, stop=True),
                 reads=["sel65", ("osb", oi)], writes=[("B", 6)])
            S.op("dve", lambda e: e.reciprocal(out=rden[oi][:], in_=B[6][0:64, :]),
                 reads=[("B", 6)], writes=[("rden", oi)])
            S.op("dve", lambda e: e.tensor_tensor(out=on[oi][:], in0=osb[oi][0:64, :], in1=rden[oi][:],
                                                  op=ALU.mult),
                 reads=[("osb", oi), ("rden", oi)], writes=[("on", oi)])

        def store_y(c, m, yi):
            S.dma("sp", f"yst{yi}", y_out[m * 64:(m + 1) * 64, c * CH:(c + 1) * CH], ych[yi][:],
                  reads=[("ych", yi)])

        def rope(bank, dst, dst_key):
            S.op("dve", lambda e: e.tensor_tensor(out=tq[:], in0=B[bank][:, :], in1=rstd[:], op=ALU.mult),
                 reads=[("B", bank), "rstd"], writes=["tq"])
            S.op("pe", lambda e: e.matmul(B[5][:, :], lhsT=rm[:], rhs=tq[:], start=True, stop=True),
                 reads=["rm", "tq"], writes=[("B", 5)])
            S.op("pool", lambda e: e.tensor_tensor(out=ta[:], in0=tq[:], in1=cosc[:], op=ALU.mult),
                 reads=["tq", "cosc"], writes=["ta"])
            S.op("dve", lambda e: e.tensor_tensor(out=tb[:], in0=B[5][:, :], in1=sinc[:], op=ALU.mult),
                 reads=[("B", 5), "sinc"], writes=["tb"])
            S.op("pool", lambda e: e.tensor_tensor(out=dst[:], in0=ta[:], in1=tb[:], op=ALU.add),
                 reads=["ta", "tb"], writes=[dst_key])

        def load_chunk(cc):
            tt = cc * CH
            for q4 in range(4):
                S.dma("sp", f"xld{q4}", xc[:, 4 * q4:4 * q4 + 4, :], xT_v[:, 4 * q4:4 * q4 + 4, tt:tt + CH],
                      writes=[("xc", q4)])
            for l in range(n_prev):
                S.dma("sp", f"ypld{l}", ypc[l][:], yp_v[l][:, :, tt:tt + CH], writes=[("ypc", l)])
            S.dma("sp", "cosld", cosc[:], cos_d[:, tt:tt + CH], writes=["cosc"])
            S.dma("sp", "sinld", sinc[:], sin_d[:, tt:tt + CH], writes=["sinc"])

        for c in range(nch):
            t0 = c * CH
            if c == 0:
                load_chunk(0)
            if n_prev:
                for f in range(KC):
                    bk = 5 + (f % 2)
                    tot = n_prev * 4
                    idx = 0
                    for l in range(n_prev):
                        for e4 in range(4):
                            S.op("pe", lambda e, l=l, e4=e4, f=f, bk=bk, idx=idx: e.matmul(
                                B[bk][:, :], lhsT=Wo[l][:, e4, f * 128:(f + 1) * 128], rhs=ypc[l][:, e4, :],
                                start=(idx == 0), stop=(idx == tot - 1)),
                                reads=[("Wo", l), ("ypc", l)], writes=[("B", bk)])
                            idx += 1
                    S.op("dve", lambda e, f=f, bk=bk: e.tensor_tensor(
                        out=xc[:, f, :], in0=B[bk][:, :], in1=xc[:, f, :], op=ALU.add),
                        reads=[("B", bk), ("xc", f // 4)], writes=[("xc", f // 4)])
            for k in range(KC):
                i3 = k % 3
                S.op("act", lambda e, k=k, i3=i3: e.activation(out=sq[i3][:], in_=xc[:, k, :], func=AF.Square),
                     reads=[("xc", k // 4)], writes=[("sq", i3)])
                S.op("pool", lambda e, k=k, i3=i3: e.tensor_copy(out=xb[i3][:], in_=xc[:, k, :]),
                     reads=[("xc", k // 4)], writes=[("xb", i3)])
                S.op("pe", lambda e, k=k, i3=i3: e.matmul(B[0][:, :], lhsT=onesb[:], rhs=sq[i3][:],
                                                         start=(k == 0), stop=(k == KC - 1)),
                     reads=["onesb", ("sq", i3)], writes=[("B", 0)])
                for g in range(4):
                    S.op("pe", lambda e, k=k, i3=i3, g=g: e.matmul(
                        B[1 + g][:, :], lhsT=Wg[:, k, g * 128:(g + 1) * 128], rhs=xb[i3][:],
                        start=(k == 0), stop=(k == KC - 1)),
                        reads=["Wg", ("xb", i3)], writes=[("B", 1 + g)])
            S.op("act", lambda e: e.activation(out=ta[:], in_=B[0][:, :], func=AF.Ln, bias=EPS,
                                               scale=1.0 / D_MODEL),
                 reads=[("B", 0)], writes=["ta"])
            S.op("act", lambda e: e.activation(out=rstd[:], in_=ta[:], func=AF.Exp, scale=-0.5),
                 reads=["ta"], writes=["rstd"])
            rope(1, qr, "qr")
            if even:
                S.op("act", lambda e: e.activation(out=Qp[0][0:64, :], in_=qr[0:64, :], func=AF.Copy),
                     reads=["qr"], writes=[("Qp", 0)])
                S.op("act", lambda e: e.activation(out=Qp[1][64:128, :], in_=qr[64:128, :], func=AF.Copy),
                     reads=["qr"], writes=[("Qp", 1)])
            else:
                S.op("act", lambda e: e.activation(out=Qp[0][0:32, :], in_=qr[0:32, :], func=AF.Copy),
                     reads=["qr"], writes=[("Qp", 0)])
                S.op("act", lambda e: e.activation(out=Qp[1][32:64, :], in_=qr[32:64, :], func=AF.Copy),
                     reads=["qr"], writes=[("Qp", 1)])
                S.op("act", lambda e: e.activation(out=Qp[2][64:128, :], in_=qr[64:128, :], func=AF.Copy,
                                                   scale=0.125),
                     reads=["qr"], writes=[("Qp", 2)])
            rope(2, kr, "kr")
            S.op("act", lambda e: e.activation(out=KT[:, t0:t0 + CH], in_=kr[:], func=AF.Copy),
                 reads=["kr"], writes=[("KT", c)])
            S.op("dve", lambda e: e.tensor_tensor(out=vbs[:], in0=B[3][:, :], in1=rstd[:], op=ALU.mult),
                 reads=[("B", 3), "rstd"], writes=["vbs"])
            for j in range(4):
                S.op("pe", lambda e, j=j: e.transpose(BT[:, j, :], vbs[:, j * 128:(j + 1) * 128], identb[:]),
                     reads=["vbs", "identb"], writes=["BT"])
            S.op("act", lambda e: e.activation(out=V1[:, 4 * c:4 * c + 4, 0:64], in_=BT[:, :, 0:64], func=AF.Copy),
                 reads=["BT"], writes=[("V1", c)])
            S.op("act", lambda e: e.activation(out=V2[:, 4 * c:4 * c + 4, 0:64], in_=BT[:, :, 64:128],
                                               func=AF.Copy),
                 reads=["BT"], writes=[("V2", c)])
            S.op("dve", lambda e: e.tensor_tensor(out=tq[:], in0=B[4][:, :], in1=rstd[:], op=ALU.mult),
                 reads=[("B", 4), "rstd"], writes=["tq"])
            S.op("act", lambda e: e.activation(out=gate[:], in_=tq[:], func=AF.Silu),
                 reads=["tq"], writes=["gate"])
            S.op("dve", lambda e: e.tensor_copy(out=gate2[:], in_=gate[64:128, :]),
                 reads=["gate"], writes=["gate2"])

            if c + 1 < nch:
                load_chunk(c + 1)

            if even:
                S.op("dve", lambda e: e.tensor_reduce(
                    out=kmT[64:128, 2 * c:2 * c + 2], in_=kr[64:128, :].rearrange("p (b t) -> p b t", t=256),
                    axis=AX.X, op=ALU.add), reads=["kr"], writes=["kmT"])
                for j in range(4):
                    cur = (4 * c + j) // 2
                    if cur <= 3:
                        S.op("dve", lambda e: e.memset(biasq[:], 0.0), writes=["biasq"])
                    else:
                        S.op("pe", lambda e, j=j: e.matmul(B[6][:, 0:64], lhsT=qr[64:128, j * 128:(j + 1) * 128],
                                                          rhs=kmT[64:128, :], start=True, stop=True),
                             reads=["qr", "kmT"], writes=[("B", 6)])
                        S.op("dve", lambda e, cur=cur: e.tensor_copy(out=gsb[:, 0:cur], in_=B[6][:, 0:cur]),
                             reads=[("B", 6)], writes=["gsb"])
                        S.op("dve", lambda e: e.max(out=max8[:], in_=gsb[:]), reads=["gsb"], writes=["max8"])
                        S.op("dve", lambda e: e.tensor_scalar(out=biasq[:], in0=gsb[:], scalar1=max8[:, 2:3],
                                                              scalar2=NEG, op0=ALU.is_lt, op1=ALU.mult),
                             reads=["gsb", "max8"], writes=["biasq"])
                        S.op("dve", lambda e, cur=cur: e.memset(biasq[:, cur:cur + 1], 0.0),
                             reads=["biasq"], writes=["biasq"])
                    S.op("pe", lambda e, j=j: e.transpose(BT[0:64, j, :], biasq[:, :], identb[:]),
                         reads=["biasq", "identb"], writes=["BT"])
                S.op("dve", lambda e: e.tensor_copy(
                    out=biasT[64:128, :], in_=BT[0:64, :, :].rearrange("p a b -> p (a b)")),
                    reads=["BT"], writes=["biasT"])

                kts = [kt for kt in range(4 * c - 16, 4 * c + 4) if kt >= 0]

                def mask_a(kt, c=c):
                    o = kt - (4 * c - 16)
                    o = o if o <= 3 else (4 if o <= 11 else o - 7)
                    return (mdil_keys, mdil[:, o, :])
                attn_softmax(c, 0, 0.125, V1, "V1", kts, mask_a, 4)
                normalize(4, 0)
                yi = state["y"] % 2
                state["y"] += 1
                S.op("dve", lambda e, yi=yi: e.tensor_tensor(out=ych[yi][:], in0=on[0][:], in1=gate[0:64, :],
                                                             op=ALU.mult),
                     reads=[("on", 0), "gate"], writes=[("ych", yi)])
                store_y(c, 0, yi)

                kts = list(range(0, 4 * c + 4))

                def mask_b(kt, c=c):
                    i = kt - 4 * c
                    if i < 0:
                        return None
                    return (["mincl"], mincl[:, i, :])
                attn_softmax(c, 1, 0.125, V2, "V2", kts, mask_b, 5, use_bias=True)
                normalize(5, 1)
                yi = state["y"] % 2
                state["y"] += 1
                S.op("dve", lambda e, yi=yi: e.tensor_tensor(out=ych[yi][:], in0=on[1][:], in1=gate2[:],
                                                             op=ALU.mult),
                     reads=[("on", 1), "gate2"], writes=[("ych", yi)])
                store_y(c, 1, yi)
            else:
                kts = list(range(0, 4 * c + 4))

                def mask_c(kt, c=c):
                    i = kt - 4 * c
                    if i < 0:
                        return None
                    return (["mincl"], mincl[:, i, :])
                sc = 1.0 / math.sqrt(32.0)
                attn_softmax(c, 0, sc, V1, "V1", kts, mask_c, 4)
                attn_softmax(c, 1, sc, V1, "V1", kts, mask_c, 5)
                normalize(4, 0)
                normalize(5, 1)
                S.op("dve", lambda e: e.scalar_tensor_tensor(out=on[0][:], in0=on[1][:], scalar=neglam[:, 0:1],
                                                             in1=on[0][:], op0=ALU.mult, op1=ALU.add),
                     reads=[("on", 0), ("on", 1), "neglam"], writes=[("on", 0)])
                S.op("act", lambda e: e.activation(out=on[1][:], in_=on[0][:], func=AF.Square),
                     reads=[("on", 0)], writes=[("on", 1)])
                S.op("pe", lambda e: e.matmul(B[6][0:64, :], lhsT=ones64[:], rhs=on[1][:], start=True, stop=True),
                     reads=["ones64", ("on", 1)], writes=[("B", 6)])
                S.op("act", lambda e: e.activation(out=rden[0][:], in_=B[6][0:64, :], func=AF.Ln, bias=EPS,
                                                   scale=1.0 / 64.0),
                     reads=[("B", 6)], writes=[("rden", 0)])
                S.op("act", lambda e: e.activation(out=rden[1][:], in_=rden[0][:], func=AF.Exp, scale=-0.5),
                     reads=[("rden", 0)], writes=[("rden", 1)])
                S.op("dve", lambda e: e.tensor_tensor(out=on[0][:], in0=on[0][:], in1=rden[1][:], op=ALU.mult),
                     reads=[("on", 0), ("rden", 1)], writes=[("on", 0)])
                yi = state["y"] % 2
                state["y"] += 1
                S.op("dve", lambda e, yi=yi: e.scalar_tensor_tensor(
                    out=ych[yi][:], in0=on[0][:], scalar=subgs[:, 0:1], in1=gate[0:64, :],
                    op0=ALU.mult, op1=ALU.mult),
                    reads=[("on", 0), "subgs", "gate"], writes=[("ych", yi)])
                store_y(c, 0, yi)

                kts = list(range(4 * c + 3, -1, -1))
                n = len(kts)
                info = {}
                for i in range(n + 2):
                    if i < n:
                        kt = kts[i]
                        sbk = i % 2
                        ei = i % 2
                        si = i % 3
                        info[i] = (kt, si)
                        S.op("pe", lambda e, kt=kt, sbk=sbk: e.matmul(
                            B[sbk][:, :], lhsT=KT[:, kt * 128:(kt + 1) * 128], rhs=Qp[2][:], start=True, stop=True),
                            reads=kt_reads(kt) + [("Qp", 2)], writes=[("B", sbk)])
                        S.op("act", lambda e, sbk=sbk, ei=ei: e.activation(out=e1[ei][:], in_=B[sbk][:, :],
                                                                         func=AF.Exp),
                             reads=[("B", sbk)], writes=[("e1", ei)])
                        S.op("act", lambda e, ei=ei, si=si: e.activation(out=spb[si][:], in_=e1[ei][:], func=AF.Ln,
                                                                       bias=1.0),
                             reads=[("e1", ei)], writes=[("spb", si)])
                        di = kt - 4 * c
                        if di >= 0:
                            S.op("dve", lambda e, si=si, di=di: e.tensor_tensor(
                                out=spb[si][:], in0=spb[si][:], in1=mstrict[:, di, :], op=ALU.mult),
                                reads=[("spb", si), "mstrict"], writes=[("spb", si)])
                    if 1 <= i <= n:
                        j = i - 1
                        kt, si = info[j]
                        lb = 2 + (j % 2)
                        S.op("pe", lambda e, si=si, lb=lb: e.matmul(B[lb][:, :], lhsT=ntri[:], rhs=spb[si][:],
                                                                    start=True, stop=False),
                             reads=["ntri", ("spb", si)], writes=[("B", lb)])
                        if j > 0:
                            S.op("pe", lambda e, lb=lb, j=j: e.matmul(B[lb][:, :], lhsT=nones[:],
                                                                      rhs=spsum[(j - 1) % 2][:],
                                                                      start=False, stop=False),
                                 reads=["nones", ("spsum", (j - 1) % 2)], writes=[("B", lb)])
                        S.op("pe", lambda e, kt=kt, lb=lb: e.matmul(
                            B[lb][:, :], lhsT=KT[:, kt * 128:(kt + 1) * 128], rhs=Qp[2][:], start=False, stop=True),
                            reads=kt_reads(kt) + [("Qp", 2)], writes=[("B", lb)])
                        if j == 0:
                            S.op("pool", lambda e, si=si: e.tensor_copy(out=spsum[0][:], in_=spb[si][:]),
                                 reads=[("spb", si)], writes=[("spsum", 0)])
                        elif j < n - 1:
                            S.op("pool", lambda e, si=si, j=j: e.tensor_tensor(
                                out=spsum[j % 2][:], in0=spsum[(j - 1) % 2][:], in1=spb[si][:], op=ALU.add),
                                reads=[("spsum", (j - 1) % 2), ("spb", si)], writes=[("spsum", j % 2)])
                        pi = state["p"] % 4
                        state["p"] += 1
                        info[j] = (kt, si, pi)
                        S.op("act", lambda e, lb=lb, pi=pi: e.activation(out=Pb[pi][:], in_=B[lb][:, :], func=AF.Exp),
                             reads=[("B", lb)], writes=[("Pb", pi)])
                        di = kt - 4 * c
                        if di >= 0:
                            S.op("dve", lambda e, pi=pi, di=di: e.tensor_tensor(
                                out=Pb[pi][:], in0=Pb[pi][:], in1=mstrict[:, di, :], op=ALU.mult),
                                reads=[("Pb", pi), "mstrict"], writes=[("Pb", pi)])
                    if i >= 2:
                        j = i - 2
                        kt, si, pi = info[j]
                        S.op("pe", lambda e, kt=kt, pi=pi, j=j: e.matmul(
                            B[4][0:65, :], lhsT=V2[:, kt, 0:65], rhs=Pb[pi][:], start=(j == 0), stop=(j == n - 1)),
                            reads=[("V2", kt // 4), "V2ones", ("Pb", pi)], writes=[("B", 4)])
                yi = state["y"] % 2
                state["y"] += 1
                S.op("dve", lambda e, yi=yi: e.tensor_tensor(out=ych[yi][:], in0=B[4][0:64, :], in1=gate2[:],
                                                             op=ALU.mult),
                     reads=[("B", 4), "gate2"], writes=[("ych", yi)])
                store_y(c, 1, yi)

        S.finish("sp")
        nc._n_inst = S.n_inst
    return nc


def build_final(n_prev, ntok, do_norm=True):
    nch = ntok // CH
    nc = bass.Bass("TRN2", target_bir_lowering=False)

    def din(name, shape, dt):
        return nc.dram_tensor(name, shape, dt, kind="ExternalInput").ap()

    xT = din("xT", [D_MODEL, ntok], F32)
    fg_d = din("fg", [128, KC], F32)
    onesb_d = din("onesb", [128, 128], BF16)
    yp_d = [din(f"yp{l}", [512, ntok], BF16) for l in range(n_prev)]
    wo_d = [din(f"wo{l}", [512, D_MODEL], F32) for l in range(n_prev)]
    o_out = nc.dram_tensor("o", [D_MODEL, ntok], F32, kind="ExternalOutput").ap()

    with contextlib.ExitStack() as st:
        def sb(name, shape, dt):
            return st.enter_context(nc.sbuf_tensor(name, shape, dt))

        def ps(name, shape, dt):
            return st.enter_context(nc.psum_tensor(name, shape, dt))

        S = Sched(nc, st)
        Wo = [sb(f"Wo{l}", [128, 4, D_MODEL], BF16) for l in range(n_prev)]
        wstage = sb("wstage", [128, 2048], F32)
        fg = sb("fg_s", [128, KC], F32)
        onesb = sb("onesb_s", [128, 128], BF16)
        xc = [sb(f"xc{i}", [128, KC, CH], F32) for i in range(2)]
        ypc = [[sb(f"ypc{l}_{i}", [128, 4, CH], BF16) for l in range(n_prev)] for i in range(2)]
        sq = [sb(f"sq{i}", [128, CH], BF16) for i in range(3)]
        lnt = sb("lnt", [128, CH], F32)
        rstd = sb("rstd", [128, CH], F32)
        B = [ps(f"B{i}", [128, 512], F32) for i in range(4)]

        S.dma("sp", "c0", fg[:], fg_d[:, :], writes=["fg"])
        S.dma("sp", "c1", onesb[:], onesb_d[:, :], writes=["onesb"])
        for l in range(n_prev):
            wo_v = wo_d[l].rearrange("(e p) n -> p e n", p=128)
            for e4 in range(4):
                S.dma("sp", "wst", wstage[:], wo_v[:, e4, :], writes=["wstage"])
                S.op("dve", lambda e, l=l, e4=e4: e.tensor_copy(out=Wo[l][:, e4, :], in_=wstage[:]),
                     reads=["wstage"], writes=[("Wo", l)])
        xT_v = xT.rearrange("(k p) t -> p k t", p=128)
        o_v = o_out.rearrange("(k p) t -> p k t", p=128)
        yp_v = [yp_d[l].rearrange("(e p) t -> p e t", p=128) for l in range(n_prev)]
        for c in range(nch):
            t0 = c * CH
            xi = c % 2
            for q4 in range(4):
                S.dma("sp", f"xld{xi}_{q4}", xc[xi][:, 4 * q4:4 * q4 + 4, :],
                      xT_v[:, 4 * q4:4 * q4 + 4, t0:t0 + CH], writes=[("xc", xi, q4)])
            for l in range(n_prev):
                S.dma("sp", f"ypld{l}_{xi}", ypc[xi][l][:], yp_v[l][:, :, t0:t0 + CH], writes=[("ypc", xi, l)])
            for f in range(KC):
                bk = 1 + (f % 2)
                tot = n_prev * 4
                idx = 0
                for l in range(n_prev):
                    for e4 in range(4):
                        S.op("pe", lambda e, l=l, e4=e4, f=f, bk=bk, idx=idx: e.matmul(
                            B[bk][:, :], lhsT=Wo[l][:, e4, f * 128:(f + 1) * 128], rhs=ypc[xi][l][:, e4, :],
                            start=(idx == 0), stop=(idx == tot - 1)),
                            reads=[("Wo", l), ("ypc", xi, l)], writes=[("B", bk)])
                        idx += 1
                S.op("dve", lambda e, f=f, bk=bk: e.tensor_tensor(
                    out=xc[xi][:, f, :], in0=B[bk][:, :], in1=xc[xi][:, f, :], op=ALU.add),
                    reads=[("B", bk), ("xc", xi, f // 4)], writes=[("xc", xi, f // 4)])
            for k in range(KC if do_norm else 0):
                i3 = k % 3
                S.op("act", lambda e, k=k, i3=i3: e.activation(out=sq[i3][:], in_=xc[xi][:, k, :], func=AF.Square),
                     reads=[("xc", xi, k // 4)], writes=[("sq", i3)])
                S.op("pe", lambda e, k=k, i3=i3: e.matmul(B[0][:, :], lhsT=onesb[:], rhs=sq[i3][:],
                                                         start=(k == 0), stop=(k == KC - 1)),
                     reads=["onesb", ("sq", i3)], writes=[("B", 0)])
            if do_norm:
                S.op("act", lambda e: e.activation(out=lnt[:], in_=B[0][:, :], func=AF.Ln, bias=EPS,
                                                   scale=1.0 / D_MODEL),
                     reads=[("B", 0)], writes=["lnt"])
                S.op("act", lambda e: e.activation(out=rstd[:], in_=lnt[:], func=AF.Exp, scale=-0.5),
                     reads=["lnt"], writes=["rstd"])
            for k in range(KC if do_norm else 0):
                eng = "dve"
                S.op(eng, lambda e, k=k: e.scalar_tensor_tensor(
                    out=xc[xi][:, k, :], in0=xc[xi][:, k, :], scalar=fg[:, k:k + 1], in1=rstd[:],
                    op0=ALU.mult, op1=ALU.mult),
                    reads=[("xc", xi, k // 4), "fg", "rstd"], writes=[("xc", xi, k // 4)])
            for q4 in range(4):
                S.dma("sp", f"ost{xi}_{q4}", o_v[:, 4 * q4:4 * q4 + 4, t0:t0 + CH],
                      xc[xi][:, 4 * q4:4 * q4 + 4, :], reads=[("xc", xi, q4)])
        S.finish("sp")
    return nc


_PROG = {}
_DEBUG_DIR = None


def _prog(key, fn):
    if key not in _PROG:
        _PROG[key] = fn()
    return _PROG[key]


def _w_cols(h):
    cols = []
    for part in range(4):
        for mixer in range(2):
            base = (mixer * 4 + part) * 256 + h * 64
            cols.extend(range(base, base + 64))
    return np.asarray(cols)


def kernel(x, norm_g, w_in, w_out, diff_lam, diff_subln_g, final_norm_g):
    x = np.asarray(x, np.float32)
    norm_g = np.asarray(norm_g, np.float32)
    w_in = np.asarray(w_in, np.float32)
    w_out = np.asarray(w_out, np.float32)
    diff_lam = np.asarray(diff_lam, np.float32)
    diff_subln_g = np.asarray(diff_subln_g, np.float32)
    final_norm_g = np.asarray(final_norm_g, np.float32)
    nb, seq, _ = x.shape
    ntok = seq // 4
    xTs = [np.ascontiguousarray(x[b].T) for b in range(nb)]
    fg = np.ascontiguousarray(final_norm_g.reshape(KC, 128).T)
    onesb = np.ones((128, 128), np.float32).astype(NPBF)
    out = None
    for layer in range(DEPTH):
        even = layer % 2 == 0
        nc = _prog(("layer", layer, seq), lambda: build_layer(layer, 0, seq))
        cos, sin, rm = _rope_tables(even, seq)
        consts = _consts(even)
        ng = np.ascontiguousarray(norm_g[layer].reshape(KC, 128).T)
        in_maps = []
        for core in range(8):
            b, h = core // 4, core % 4
            m = {"xT": xTs[b], "w": np.ascontiguousarray(w_in[layer][:, _w_cols(h)]), "ng": ng,
                 "cos": cos, "sin": sin, "rm": rm}
            m.update(consts)
            if not even:
                m["lam"] = np.ascontiguousarray(diff_lam[layer // 2].reshape(1, 128))
                m["subg"] = np.ascontiguousarray(diff_subln_g[layer // 2].reshape(64, 1))
            in_maps.append(m)
        res = run_bass_kernel_spmd(nc, in_maps, core_ids=list(range(8)))
        ylay = []
        for b in range(nb):
            yb = np.empty((512, seq), NPBF)
            for h in range(4):
                r = np.asarray(res.results[b * 4 + h]["y"])
                yb[h * 64:(h + 1) * 64] = r[0:64]
                yb[256 + h * 64:256 + (h + 1) * 64] = r[64:128]
            ylay.append(yb)
        if _DEBUG_DIR:
            np.save(f"{_DEBUG_DIR}/dbg_y{layer}.npy", np.stack(ylay).view(np.uint16))
        last = layer == DEPTH - 1
        ncu = _prog(("upd", ntok, last), lambda: build_final(1, ntok, do_norm=last))
        in_maps = []
        for core in range(8):
            b, j = core // 4, core % 4
            sl = slice(j * ntok, (j + 1) * ntok)
            in_maps.append({"xT": np.ascontiguousarray(xTs[b][:, sl]), "fg": fg, "onesb": onesb,
                            "yp0": np.ascontiguousarray(ylay[b][:, sl]), "wo0": w_out[layer]})
        res = run_bass_kernel_spmd(ncu, in_maps, core_ids=list(range(8)))
        if not last:
            for core in range(8):
                b, j = core // 4, core % 4
                xTs[b][:, j * ntok:(j + 1) * ntok] = np.asarray(res.results[core]["o"])
        else:
            out = np.empty((nb, seq, D_MODEL), np.float32)
            for core in range(8):
                b, j = core // 4, core % 4
                out[b, j * ntok:(j + 1) * ntok, :] = np.asarray(res.results[core]["o"]).T
    return out
```
